# Optimizing a Trainium2 kernel written in Bass

```python
import math
import jax, jax.numpy as jnp
from jax import lax
import numpy as np

D_MODEL = 1024
BATCH = 8
SEQ = 4096
DEPTH = 2

ATTN_HEADS = 8
HEAD_DIM = 64
ATTN_WIDTH = ATTN_HEADS * HEAD_DIM
Q_BLOCK = 128
POOL_WINDOWS = (2, 4, 8, 16)
POOL_GROUPS = len(POOL_WINDOWS)
POOL_WIDTH = D_MODEL - ATTN_WIDTH
POOL_GROUP_DIM = POOL_WIDTH // POOL_GROUPS
IN_WIDTH = 3 * ATTN_WIDTH + ATTN_HEADS + POOL_WIDTH
N_EXPERT_GROUPS = 4
EXPERTS_PER_GROUP = 8
TOP_K_EXPERT = 2
D_EXPERT = 256
N_MOD = 6
EPS = 1e-6
NEG_INF = -1e30
FGATE_BIAS_INIT = 3.0

kernel_name = "hybrid_fox_pool_hmoe_adaln"


def rmsnorm(x, g):
    xf = x.astype(jnp.float32)
    y = xf * lax.rsqrt(jnp.mean(xf * xf, axis=-1, keepdims=True) + EPS)
    return (y * g.astype(jnp.float32)).astype(x.dtype)


def modulate(h, shift, scale):
    return h * (1.0 + scale[:, None, :]) + shift[:, None, :]


def forgetting_attention(q, k, v, log_f):
    B, S, H, dh = q.shape
    nb = S // Q_BLOCK
    qh = q.transpose(0, 2, 1, 3)
    kh = k.transpose(0, 2, 1, 3)
    vh = v.transpose(0, 2, 1, 3)
    F = jnp.cumsum(log_f, axis=1).transpose(0, 2, 1)
    q_blocks = qh.reshape(B, H, nb, Q_BLOCK, dh).transpose(2, 0, 1, 3, 4)
    fq_blocks = F.reshape(B, H, nb, Q_BLOCK).transpose(2, 0, 1, 3)
    starts = jnp.arange(nb, dtype=jnp.int32) * Q_BLOCK
    key_pos = jnp.arange(S, dtype=jnp.int32)
    scale = 1.0 / math.sqrt(dh)

    def one_block(args):
        qb, fqb, start = args
        s = jnp.einsum('bhqd,bhkd->bhqk', qb, kh).astype(jnp.float32) * scale
        s = s + fqb[..., None] - F[:, :, None, :]
        qpos = start + jnp.arange(Q_BLOCK, dtype=jnp.int32)
        causal = key_pos[None, :] <= qpos[:, None]
        s = jnp.where(causal[None, None], s, NEG_INF)
        p = jax.nn.softmax(s, axis=-1).astype(vh.dtype)
        return jnp.einsum('bhqk,bhkd->bhqd', p, vh)

    out = lax.map(one_block, (q_blocks, fq_blocks, starts))
    return out.transpose(1, 0, 3, 2, 4).reshape(B, S, H * dh)


def trailing_mean(cs, w):
    S = cs.shape[1]
    prev = jnp.pad(cs[:, :S - w], ((0, 0), (w, 0), (0, 0)))
    cnt = jnp.minimum(jnp.arange(1, S + 1), w).astype(jnp.float32)
    return (cs - prev) / cnt[None, :, None]


def multiscale_pool(u, w_pool, pool_scale):
    B, S, _ = u.shape
    ug = u.reshape(B, S, POOL_GROUPS, POOL_GROUP_DIM)
    cs = jnp.cumsum(ug.astype(jnp.float32), axis=1)
    pooled = jnp.stack(
        [trailing_mean(cs[:, :, g, :], w) for g, w in enumerate(POOL_WINDOWS)], axis=2)
    diff = (pooled - ug.astype(jnp.float32)).astype(u.dtype)
    mixed = jnp.einsum('bsgc,gcd->bsgd', diff, w_pool)
    return mixed.reshape(B, S, POOL_WIDTH) * pool_scale


def hierarchical_moe(h, w_rg, b_rg, w_re, b_re, w_gate, w_up, w_down):
    lg = jnp.matmul(h, w_rg).astype(jnp.float32) + b_rg
    pg = jax.nn.softmax(lg, axis=-1)
    top_p, top_g = lax.top_k(pg, 1)
    le = jnp.einsum('td,gde->tge', h, w_re).astype(jnp.float32) + b_re
    le_sel = jnp.take_along_axis(le, top_g[:, :, None], axis=1)[:, 0]
    ev, ei = lax.top_k(le_sel, TOP_K_EXPERT)
    ew = jax.nn.softmax(ev, axis=-1)
    e_comb = jnp.sum(jax.nn.one_hot(ei, EXPERTS_PER_GROUP, dtype=jnp.float32) * ew[..., None], axis=1)
    g_comb = jax.nn.one_hot(top_g[:, 0], N_EXPERT_GROUPS, dtype=jnp.float32) * top_p
    comb = (g_comb[:, :, None] * e_comb[:, None, :]).astype(h.dtype)
    y = jnp.zeros_like(h)
    for g in range(N_EXPERT_GROUPS):
        a = jnp.einsum('td,edf->tef', h, w_gate[g])
        b = jnp.einsum('td,edf->tef', h, w_up[g])
        act = jax.nn.silu(a) * b * comb[:, g, :, None]
        y = y + jnp.einsum('tef,efd->td', act, w_down[g])
    return y


def setup_inputs(seed: int = 0) -> dict:
    key = jax.random.key(seed)
    ks = jax.random.split(key, 20)
    f32 = jnp.float32
    D, L = D_MODEL, DEPTH
    G, E, F = N_EXPERT_GROUPS, EXPERTS_PER_GROUP, D_EXPERT

    def nrm(k, shape, scale):
        return jax.random.normal(k, shape, f32) * scale

    return {
        "x": nrm(ks[0], (BATCH, SEQ, D), 1.0),
        "c": nrm(ks[1], (BATCH, D), 1.0),
        "norm_mix_g": 1.0 + nrm(ks[2], (L, D), 0.02),
        "norm_ffn_g": 1.0 + nrm(ks[3], (L, D), 0.02),
        "norm_final_g": 1.0 + nrm(ks[4], (D,), 0.02),
        "w_ada": nrm(ks[5], (L, D, N_MOD * D), 0.5 * D ** -0.5),
        "b_ada": nrm(ks[6], (L, N_MOD * D), 0.02),
        "w_in": nrm(ks[7], (L, D, IN_WIDTH), D ** -0.5),
        "b_fgate": FGATE_BIAS_INIT + nrm(ks[8], (L, ATTN_HEADS), 0.1),
        "w_pool": nrm(ks[9], (L, POOL_GROUPS, POOL_GROUP_DIM, POOL_GROUP_DIM), POOL_GROUP_DIM ** -0.5),
        "pool_scale": 1.0 + nrm(ks[10], (L, POOL_WIDTH), 0.1),
        "w_out": nrm(ks[11], (L, D, D), D ** -0.5),
        "w_router_group": nrm(ks[12], (L, D, G), D ** -0.5),
        "b_router_group": nrm(ks[13], (L, G), 0.01),
        "w_router_expert": nrm(ks[14], (L, G, D, E), D ** -0.5),
        "b_router_expert": nrm(ks[15], (L, G, E), 0.01),
        "w_expert_gate": nrm(ks[16], (L, G, E, D, F), D ** -0.5),
        "w_expert_up": nrm(ks[17], (L, G, E, D, F), D ** -0.5),
        "w_expert_down": nrm(ks[18], (L, G, E, F, D), F ** -0.5),
    }


def reference(x, c, norm_mix_g, norm_ffn_g, norm_final_g, w_ada, b_ada, w_in, b_fgate,
              w_pool, pool_scale, w_out, w_router_group, b_router_group, w_router_expert,
              b_router_expert, w_expert_gate, w_expert_up, w_expert_down):
    B, S, D = x.shape
    split_pts = [ATTN_WIDTH, 2 * ATTN_WIDTH, 3 * ATTN_WIDTH, 3 * ATTN_WIDTH + ATTN_HEADS]
    c_act = jax.nn.silu(c)
    for l in range(DEPTH):
        mod = jnp.matmul(c_act, w_ada[l]) + b_ada[l]
        sh_m, sc_m, gt_m, sh_f, sc_f, gt_f = jnp.split(mod, N_MOD, axis=-1)

        h = modulate(rmsnorm(x, norm_mix_g[l]), sh_m, sc_m)
        proj = jnp.matmul(h, w_in[l])
        q, k, v, f_logit, u = jnp.split(proj, split_pts, axis=-1)
        q = q.reshape(B, S, ATTN_HEADS, HEAD_DIM)
        k = k.reshape(B, S, ATTN_HEADS, HEAD_DIM)
        v = v.reshape(B, S, ATTN_HEADS, HEAD_DIM)
        log_f = jax.nn.log_sigmoid(f_logit.astype(jnp.float32) + b_fgate[l].astype(jnp.float32))
        attn_out = forgetting_attention(q, k, v, log_f)
        pool_out = multiscale_pool(u, w_pool[l], pool_scale[l])
        mix = jnp.matmul(jnp.concatenate([attn_out, pool_out.astype(attn_out.dtype)], axis=-1), w_out[l])
        x = x + gt_m[:, None, :] * mix

        h = modulate(rmsnorm(x, norm_ffn_g[l]), sh_f, sc_f)
        ffn = hierarchical_moe(h.reshape(B * S, D), w_router_group[l], b_router_group[l],
                               w_router_expert[l], b_router_expert[l], w_expert_gate[l],
                               w_expert_up[l], w_expert_down[l])
        x = x + gt_f[:, None, :] * ffn.reshape(B, S, D)
    return rmsnorm(x, norm_final_g)
```

```python
import contextlib
import numpy as np
import concourse.bass as bass
import concourse.mybir as mybir
from concourse.bass_utils import run_bass_kernel_spmd

F32 = mybir.dt.float32
BF16 = mybir.dt.bfloat16
I32 = mybir.dt.int32
ALU = mybir.AluOpType
AF = mybir.ActivationFunctionType
AX = mybir.AxisListType

S = 4096
D = 1024
NT = 32
NL = 2
NH = 8
NTILE = 96
EPS = 1e-6
BIG = 30000.0
POOL_W = (2, 4, 8, 16)

COMPUTE = ("pe", "act", "dve", "pool")
DMAQ = ("sp", "pool", "act")


class _Rec:
    def __init__(self):
        self.call = None

    def __getattr__(self, name):
        def f(*a, **kw):
            assert self.call is None
            self.call = (name, a, kw)
            return self
        return f


class _Late:
    def __init__(self):
        self.v = None


def _replay(fn):
    rec = _Rec()
    fn(rec)
    name, a, kw = rec.call
    return lambda e: getattr(e, name)(*a, **{k: (v.v if isinstance(v, _Late) else v) for k, v in kw.items()})


class _Op:
    __slots__ = ("eng", "fn", "deps", "is_dma", "sig", "has_dep", "bg")

    def __init__(self, eng, fn, is_dma, bg):
        self.eng = eng
        self.fn = _replay(fn)
        self.deps = set()
        self.is_dma = is_dma
        self.sig = None
        self.has_dep = False
        self.bg = bg


class Prog:
    def __init__(self, nc, dma_ring=8):
        self.nc = nc
        self.ops = []
        self.last_writer = {}
        self.readers = {}
        self.dma_ring = dma_ring
        self.fence_op = None
        self.since_fence_dma = []
        self.last_eng_op = {}
        self.wbound = _Late()

    def _add(self, eng, fn, reads, writes, is_dma, bg):
        o = _Op(eng, fn, is_dma, bg)
        for k in reads:
            w = self.last_writer.get(k)
            if w is not None:
                o.deps.add(w)
        for k in writes:
            w = self.last_writer.get(k)
            if w is not None:
                o.deps.add(w)
            for r in self.readers.get(k, ()):
                o.deps.add(r)
        for k in writes:
            self.last_writer[k] = o
            self.readers[k] = []
        for k in reads:
            self.readers.setdefault(k, []).append(o)
        if not bg:
            if self.fence_op is not None:
                o.deps.add(self.fence_op)
            if is_dma:
                self.since_fence_dma.append(o)
            else:
                self.last_eng_op[eng] = o
        o.deps.discard(o)
        self.ops.append(o)
        return o

    def op(self, eng, fn, reads=(), writes=(), bg=False):
        return self._add(eng, fn, reads, writes, False, bg)

    def dma(self, eng, fn, reads=(), writes=(), bg=False):
        return self._add(eng, fn, reads, writes, True, bg)

    def fence(self, scratch):
        o = _Op("pool", lambda e: e.memset(scratch, 0.0), False, False)
        if self.fence_op is not None:
            o.deps.add(self.fence_op)
        for d in self.since_fence_dma:
            o.deps.add(d)
        for d in self.last_eng_op.values():
            o.deps.add(d)
        self.since_fence_dma = []
        self.last_eng_op = {"pool": o}
        self.fence_op = o
        self.ops.append(o)
        return o

    def emit(self):
        nc = self.nc
        ops = self.ops
        for o in ops:
            for d in o.deps:
                if d.is_dma:
                    d.has_dep = True
                elif d.eng == o.eng and d.eng == "pe" and not o.is_dma:
                    pass
                else:
                    d.has_dep = True
        stack = contextlib.ExitStack()
        sems = {e: stack.enter_context(nc.semaphore("s_" + e)) for e in COMPUTE}
        dsem = {q: [stack.enter_context(nc.semaphore("d_%s%d" % (q, i))) for i in range(self.dma_ring)]
                for q in DMAQ}
        cnt = {e: 0 for e in COMPUTE}
        dcnt = {q: [0] * self.dma_ring for q in DMAQ}
        dnum = {q: 0 for q in DMAQ}
        engs = ("pe", "act", "dve", "pool", "sp")
        waited = {e: {} for e in engs}
        streams = {e: [] for e in engs}
        for o in ops:
            eng = o.eng
            need = {}
            for d in o.deps:
                if d.sig is None:
                    continue
                if (not d.is_dma) and d.eng == eng and eng == "pe" and not o.is_dma:
                    continue
                s, v = d.sig
                if need.get(s, (None, 0))[1] < v:
                    need[s] = (s, v)
            if o.is_dma:
                r = dnum[eng] % self.dma_ring
                dnum[eng] += 1
                s = dsem[eng][r]
                prev = dcnt[eng][r]
                if prev > 0 and need.get(s, (None, 0))[1] < prev:
                    need[s] = (s, prev)
                dcnt[eng][r] += 16
                o.sig = (s, dcnt[eng][r])
                inc = (s, 16)
            elif o.has_dep:
                cnt[eng] += 1
                o.sig = (sems[eng], cnt[eng])
                inc = (sems[eng], 1)
            else:
                inc = None
            w = waited[eng]
            wl = []
            for s, v in need.values():
                if w.get(s, 0) >= v:
                    continue
                w[s] = v
                wl.append((s, v))
            streams[eng].append((wl, o.fn, inc))
        fin = []
        for q in DMAQ:
            for i in range(self.dma_ring):
                if dcnt[q][i] > 0:
                    fin.append((dsem[q][i], dcnt[q][i]))
        for e in COMPUTE:
            if cnt[e] > 0:
                fin.append((sems[e], cnt[e]))
        self.n_instr = {e: len(v) for e, v in streams.items()}

        def run(engname, e):
            if engname == "pool":
                r = e.alloc_register("wbound")
                e.reg_mov(r, 4095)
                self.wbound.v = r
            for wl, fn, inc in streams[engname]:
                for s, v in wl:
                    e.wait_ge(s, v)
                ins = fn(e)
                if inc is not None:
                    ins.then_inc(inc[0], inc[1])
            if engname == "sp":
                for s, v in fin:
                    e.wait_ge(s, v)

        with nc.Block() as block:
            @block.tensor
            def _(e):
                run("pe", e)

            @block.scalar
            def _(e):
                run("act", e)

            @block.vector
            def _(e):
                run("dve", e)

            @block.gpsimd
            def _(e):
                run("pool", e)

            @block.sync
            def _(e):
                run("sp", e)
        stack.close()


def make_consts():
    c = {}
    idx = np.arange(128)
    s_ = idx[:, None]
    t_ = idx[None, :]
    c["ident"] = np.eye(128, dtype=np.float32)
    c["triu"] = (s_ <= t_).astype(np.float32)
    c["ltri"] = (s_ < t_).astype(np.float32)
    c["ones"] = np.ones((128, 128), np.float32)
    c["negmask"] = np.where(t_ > s_, -BIG, 0.0).astype(np.float32)
    band = np.zeros((12, 128, 128), np.float32)
    invc = np.zeros((4, 128, 128), np.float32)
    for g, w in enumerate(POOL_W):
        same = ((s_ <= t_) & (s_ > t_ - w)).astype(np.float32)
        same[idx, idx] -= w
        prev = (s_ - 128 > t_ - w).astype(np.float32)
        first = ((s_ <= t_) & (s_ > t_ - w)).astype(np.float32)
        cntv = np.minimum(idx + 1, w).astype(np.float32)
        first[idx, idx] -= cntv
        band[g * 3 + 0] = same
        band[g * 3 + 1] = prev
        band[g * 3 + 2] = first
        invc[g] = np.broadcast_to((1.0 / cntv)[None, :], (128, 128))
    c["band"] = band
    c["invc"] = invc
    c["jv"] = np.broadcast_to((np.arange(NTILE, dtype=np.float32) * 128.0)[None, :], (128, NTILE)).copy()
    c["pidx"] = np.arange(128, dtype=np.float32)[:, None].copy()
    return c


CONST_SHAPES = {"ident": [128, 128], "triu": [128, 128], "ltri": [128, 128], "ones": [128, 128],
                "negmask": [128, 128], "band": [12, 128, 128], "invc": [4, 128, 128],
                "jv": [128, NTILE], "pidx": [128, 1]}

IN_SHAPES = {
    "x": [S, D], "c_col": [128, 8], "norm_mix_g": [NL, D], "norm_ffn_g": [NL, D], "norm_final_g": [1, D],
    "w_ada": [NL, D, 6 * D], "b_ada": [NL, 6 * D], "w_in": [NL, D, 2056], "b_fgate": [NL, 8],
    "w_pool": [NL, 4, 128, 128], "ps_col": [NL, 128, 4], "w_out": [NL, D, D],
    "w_r": [NL, D, 36], "b_r": [NL, 36], "wexp": [NL, 4096, 6144],
}


def build(nl=NL, stop=None, dbg=()):
    nc = bass.Bass("TRN2", target_bir_lowering=False)
    st = contextlib.ExitStack()
    din = {}
    need_in = set(IN_SHAPES)
    if stop in ("p1", "p2"):
        need_in -= {"w_out", "w_r", "b_r", "wexp", "norm_final_g"}
    if stop == "p3":
        need_in -= {"wexp", "norm_final_g"}
    for k in sorted(need_in):
        din[k] = nc.dram_tensor(k, IN_SHAPES[k], F32, kind="ExternalInput").ap()
    for k, shp in CONST_SHAPES.items():
        din["k_" + k] = nc.dram_tensor("k_" + k, shp, F32, kind="ExternalInput").ap()
    out_d = nc.dram_tensor("out", [S, D], F32, kind="ExternalOutput").ap()

    def scratch(name, shape, dt):
        kind = "ExternalOutput" if name in dbg else "Internal"
        return nc.dram_tensor(name, shape, dt, kind=kind).ap()

    xres = scratch("xres", [S, D], F32)
    qT_d = scratch("qT_d", [NH, 65, S], BF16)
    kT_d = scratch("kT_d", [NH, 65, S], BF16)
    v_d = scratch("v_d", [S, 512], BF16)
    h_d = scratch("h_d", [S, D], BF16)
    hs_d = scratch("hs_d", [NTILE * 128, D], BF16)
    ys_d = scratch("ys_d", [NTILE * 128, D], F32)
    wbf_d = [scratch("wbf%d" % l, [4096, 6144], BF16) for l in range(nl)]
    dbg_d = {}
    for name, shp, dt in (("dbg_cat", [128, 8, S], BF16), ("dbg_G", [128, 8, NT], F32),
                          ("dbg_mod", [128, 6 * D], F32), ("dbg_LG", [128, NT, 36], F32),
                          ("dbg_route", [128, 6, NT], F32), ("dbg_te", [128, NTILE], F32)):
        if name in dbg:
            dbg_d[name] = nc.dram_tensor(name, shp, dt, kind="ExternalOutput").ap()

    def T(name, shape, dt):
        return st.enter_context(nc.sbuf_tensor(name, shape, dt))

    pg = Prog(nc)
    psb = [st.enter_context(nc.psum_tensor("psb%d" % i, [128, 512], F32)) for i in range(8)]
    ps_rr = [0]

    def ps_next():
        b = ps_rr[0] % 8
        ps_rr[0] += 1
        return psb[b], ("ps", b)

    arenaA = T("arenaA", [128, 8 * S], BF16)
    arenaB = T("arenaB", [128, 8 * 2056], BF16)
    mod = T("mod", [128, 6 * D], F32)
    vaug = [T("vaug%d" % i, [128, NT, 128], BF16) for i in range(2)]
    stage = [T("stage%d" % i, [128, 2048], BF16) for i in range(2)]
    xt = [T("xt%d" % i, [128, D], F32) for i in range(3)]
    tmpf = [T("tmpf%d" % i, [128, D], F32) for i in range(2)]
    xnt = [T("xnt%d" % i, [128, D], F32) for i in range(2)]
    hb = [T("hb%d" % i, [128, D], BF16) for i in range(2)]
    sq = T("sq", [128, D], BF16)
    fsc = T("fsc", [128, 8], F32)
    ident_f = T("ident_f", [128, 128], F32)
    triu_f = T("triu_f", [128, 128], F32)
    ones_f = T("ones_f", [128, 128], F32)
    ident_b = T("ident_b", [128, 128], BF16)
    ltri_b = T("ltri_b", [128, 128], BF16)
    ones_b = T("ones_b", [128, 128], BF16)
    negm_b = T("negm_b", [128, 128], BF16)
    band_b = T("band_b", [128, 12, 128], BF16)
    invc_f = T("invc_f", [128, 4, 128], F32)
    jv_f = T("jv_f", [128, NTILE], F32)
    pidx_f = T("pidx_f", [128, 1], F32)
    zero_b = T("zero_b", [128, 128], BF16)
    epsc = T("epsc", [128, 1], F32)
    cb = T("cb", [128, 8, 128], BF16)
    ccol = T("ccol", [128, 8], F32)
    cact = T("cact", [128, 8], F32)
    ssq = T("ssq", [128, NT], F32)
    rstd = T("rstd", [128, NT], F32)
    zf = T("zf", [128, 8, NT], F32)
    Gm = T("Gm", [128, 8, NT], F32)
    fA = T("fA", [128, 8, NT], F32)
    fB = T("fB", [128, 8, NT], F32)
    ftot = T("ftot", [128, 8, NT], F32)
    bfg = T("bfg", [128, 8], F32)
    gtn = [T("gtn%d" % i, [128, 128], BF16) for i in range(2)]
    c8 = arenaA[0:8, 0:S]
    pscol = T("pscol", [128, 4], F32)
    wpool_b = T("wpool_b", [128, 4, 128], BF16)
    gb = arenaA[:, 16384:18432].bitcast(F32)
    bada = [arenaA[:, 8192 + i * 1024:8192 + (i + 1) * 1024].bitcast(F32) for i in range(2)]
    wada = [arenaA[:, i * 4096:(i + 1) * 4096].rearrange("p (k n) -> p k n", k=8) for i in range(2)]
    LG = T("LG", [128, NT, 36], F32)
    brb = T("brb", [128, 36], F32)
    wr_b = T("wr_b", [128, 8, 36], BF16)
    pos_i = [T("pos_i%d" % k, [128, NT], I32) for k in range(2)]
    comb = [T("comb%d" % k, [128, NT], F32) for k in range(2)]
    idxW = T("idxW", [128, NTILE], I32)
    rrow = T("rrow", [128, 512], F32)
    rbs = T("rbs", [128, 512], F32)

    def load_const(dst, src, cast, key):
        q = "pool" if cast else "sp"
        pg.dma(q, lambda e: e.dma_start(out=dst, in_=src), writes=[key])

    load_const(ident_f[:], din["k_ident"], False, "ident_f")
    load_const(triu_f[:], din["k_triu"], False, "triu_f")
    load_const(ones_f[:], din["k_ones"], False, "ones_f")
    load_const(ident_b[:], din["k_ident"], True, "ident_b")
    load_const(ltri_b[:], din["k_ltri"], True, "ltri_b")
    load_const(ones_b[:], din["k_ones"], True, "ones_b")
    load_const(negm_b[:], din["k_negmask"], True, "negm_b")
    load_const(band_b[:], din["k_band"].rearrange("g s t -> s g t"), True, "band_b")
    load_const(invc_f[:], din["k_invc"].rearrange("g s t -> s g t"), False, "invc_f")
    load_const(jv_f[:], din["k_jv"], False, "jv_f")
    load_const(pidx_f[:], din["k_pidx"], False, "pidx_f")
    pg.op("pool", lambda e: e.memset(zero_b[:], 0.0), writes=["zero_b"])
    pg.op("pool", lambda e: e.memset(epsc[:], EPS), writes=["epsc"])
    pg.op("pool", lambda e: e.memset(c8, 8.0), writes=["c8"])
    pg.dma("sp", lambda e: e.dma_start(out=kT_d[:, 64, :], in_=c8), reads=["c8"], writes=["kaug"])
    for i in range(2):
        odd = i % 2
        pg.op("pool", lambda e, i=i: e.memset(vaug[i][:], 0.0), writes=[("vaug", i)])
        col = 0 if odd else 64
        pg.op("pool", lambda e, i=i, col=col: e.memset(vaug[i][:, :, col:col + 64], 1.0), writes=[("vaug", i)])
    pg.dma("sp", lambda e: e.dma_start(out=ccol[:], in_=din["c_col"]), writes=["ccol"])
    pg.op("act", lambda e: e.activation(cact[:], ccol[:], AF.Silu), reads=["ccol"], writes=["cact"])
    for k in range(8):
        pg.op("act", lambda e, k=k: e.activation(cb[:, k, :], zero_b[:], AF.Identity, bias=cact[:, k:k + 1], scale=1.0),
              reads=["zero_b", "cact"], writes=["cb"])

    conv_state = {"n": 0}
    conv_items = []
    if stop is None or stop == "p4":
        for l in range(nl):
            for e_ in range(32):
                for piece in range(3):
                    conv_items.append((l, e_, piece))

    def conv_step(nsteps=1):
        for _ in range(nsteps):
            if not conv_items:
                return
            l, e_, piece = conv_items.pop(0)
            n = conv_state["n"]
            conv_state["n"] += 1
            sl = n % 2
            src = din["wexp"][l, e_ * 128:(e_ + 1) * 128, piece * 2048:(piece + 1) * 2048]
            dst = wbf_d[l][e_ * 128:(e_ + 1) * 128, piece * 2048:(piece + 1) * 2048]
            pg.dma("pool", lambda e, sl=sl, src=src: e.dma_start(out=stage[sl][:], in_=src),
                   writes=[("stage", sl)], bg=True)
            pg.dma("sp", lambda e, sl=sl, dst=dst: e.dma_start(out=dst, in_=stage[sl][:]),
                   reads=[("stage", sl)], writes=[("wbf", l, e_, piece)], bg=True)

    def rms_sq(xs, xkey, i):
        pg.op("act", lambda e: e.activation(sq[:], xs, AF.Square, accum_out=ssq[:, i:i + 1]),
              reads=[xkey], writes=["sq", ("ssq", i)])
        pg.op("act", lambda e: e.activation(rstd[:, i:i + 1], ssq[:, i:i + 1], AF.Ln, bias=epsc[:, 0:1], scale=1.0 / D),
              reads=[("ssq", i), "epsc"], writes=[("rstd", i)])
        pg.op("act", lambda e: e.activation(rstd[:, i:i + 1], rstd[:, i:i + 1], AF.Exp, scale=-0.5), reads=[("rstd", i)], writes=[("rstd", i)])

    def rms_apply(xs, xkey, i, Gap, SHap, hdst, hkey, tslot):
        tm = tmpf[tslot]
        pg.op("dve", lambda e: e.scalar_tensor_tensor(tm[:], xs, rstd[:, i:i + 1], Gap, ALU.mult, ALU.mult),
              reads=[xkey, ("rstd", i), "mod"], writes=[("tmpf", tslot)])
        pg.op("pool", lambda e: e.tensor_tensor(hdst, tm[:], SHap, ALU.add),
              reads=[("tmpf", tslot), "mod"], writes=[hkey])

    catT = arenaA[:].rearrange("p (c t) -> p c t", c=8)

    NMC = 24
    mod_d = scratch("mod_d", [1, 6 * D], F32)
    mwc = [xt[i][:].bitcast(BF16).rearrange("p (k n) -> p k n", k=8) for i in range(3)]
    mbc = [xnt[0][0:1, i * 256:(i + 1) * 256] for i in range(2)]
    msg = [xnt[1][0:1, i * 256:(i + 1) * 256] for i in range(2)]

    def m_load(l1, c):
        pg.dma("pool", lambda e: e.dma_start(
            out=mwc[c % 3], in_=din["w_ada"][l1].rearrange("(k p) n -> p k n", p=128)[:, :, c * 256:(c + 1) * 256]),
            writes=[("mwc", c % 3)])
        pg.dma("sp", lambda e: e.dma_start(out=mbc[c % 2], in_=din["b_ada"][l1:l1 + 1, c * 256:(c + 1) * 256]),
               writes=[("mbc", c % 2)])

    def m_comp(c, bank):
        pb = psb[bank]
        for k in range(8):
            pg.op("pe", lambda e: e.matmul(pb[:, 0:256], cb[:, k, :], mwc[c % 3][:, k, :], start=(k == 0), stop=(k == 7)),
                  reads=["cb", ("mwc", c % 3)], writes=[("ps", bank)])
        pg.op("dve", lambda e: e.tensor_tensor(msg[c % 2], pb[0:1, 0:256], mbc[c % 2], ALU.add),
              reads=[("ps", bank), ("mbc", c % 2)], writes=[("msg", c % 2)])
        pg.dma("sp", lambda e: e.dma_start(out=mod_d[0:1, c * 256:(c + 1) * 256], in_=msg[c % 2]),
               reads=[("msg", c % 2)], writes=[("mod_d", c)])

    for l in range(nl):
        xsrc = din["x"] if l == 0 else xres
        pg.fence(fsc[:, 0:1])
        if l == 0:
            wst = [arenaA[:, 16384 + i * 8192:16384 + (i + 1) * 8192].bitcast(F32).rearrange("p (k n) -> p k n", k=8) for i in range(2)]

            def wst_load(n):
                pg.dma("sp" if n % 2 == 0 else "pool", lambda e: e.dma_start(
                    out=wst[n % 2], in_=din["w_ada"][l].rearrange("(k p) n -> p k n", p=128)[:, :, n * 512:(n + 1) * 512]),
                    writes=[("wst", n % 2)])
            wst_load(0)
            wst_load(1)
            for n in range(12):
                sl = n % 2
                pg.dma("sp", lambda e: e.dma_start(
                    out=bada[sl], in_=din["b_ada"][l:l + 1, n * 512:(n + 1) * 512].partition_broadcast(128)[:, 0, :]),
                    writes=[("bada", sl)])
                pg.op("act", lambda e: e.activation(wada[sl][:, 0:4, :], wst[sl][:, 0:4, :], AF.Copy),
                      reads=[("wst", sl)], writes=[("wada", sl, 0)])
                pg.op("dve", lambda e: e.tensor_copy(wada[sl][:, 4:8, :], wst[sl][:, 4:8, :]),
                      reads=[("wst", sl)], writes=[("wada", sl, 1)])
                if n + 2 < 12:
                    wst_load(n + 2)
                pb, pk = ps_next()
                for k in range(8):
                    pg.op("pe", lambda e: e.matmul(pb[:], cb[:, k, :], wada[sl][:, k, :], start=(k == 0), stop=(k == 7)),
                          reads=["cb", ("wada", sl, k // 4)], writes=[pk])
                pg.op("dve", lambda e: e.tensor_tensor(mod[:, n * 512:(n + 1) * 512], pb[:], bada[sl], ALU.add),
                      reads=[pk, ("bada", sl)], writes=["mod"])
        def mod_gains(l_):
            for (goff, gname) in ((1024, "norm_mix_g"), (4096, "norm_ffn_g")):
                pg.dma("sp", lambda e: e.dma_start(out=gb, in_=din[gname][l_:l_ + 1, :].partition_broadcast(128)[:, 0, :]),
                       writes=["gb", ("wst", 0)])
                pg.op("dve", lambda e: e.scalar_tensor_tensor(mod[:, goff:goff + D], mod[:, goff:goff + D], 1.0, gb, ALU.add, ALU.mult),
                      reads=["gb", "mod"], writes=["mod"])

        def mod_reload(l_):
            for q in range(4):
                pg.dma("sp" if q % 2 == 0 else "pool", lambda e: e.dma_start(
                    out=mod[:, q * 1536:(q + 1) * 1536], in_=mod_d[0:1, q * 1536:(q + 1) * 1536].partition_broadcast(128)[:, 0, :]),
                    reads=[("mod_d", c) for c in range(NMC)] + ["mod"], writes=["mod"])
            mod_gains(l_)
        if l == 0:
            mod_gains(l)
        SH1, G1, GTM = mod[:, 0:D], mod[:, D:2 * D], mod[:, 2 * D:3 * D]
        SH2, G2, GTF = mod[:, 3 * D:4 * D], mod[:, 4 * D:5 * D], mod[:, 5 * D:6 * D]
        if "dbg_mod" in dbg_d and l == 0:
            pg.dma("sp", lambda e: e.dma_start(out=dbg_d["dbg_mod"], in_=mod[:]), reads=["mod"], writes=["dbgmod"])

        w_in_b = arenaB[:].rearrange("p (k n) -> p k n", k=8)
        wsrc = din["w_in"][l].rearrange("(k p) n -> p k n", p=128)
        pg.dma("pool", lambda e: e.dma_start(out=w_in_b[:, :, 0:1024], in_=wsrc[:, :, 0:1024]), writes=["w_in_b"])
        pg.dma("pool", lambda e: e.dma_start(out=w_in_b[:, :, 1024:2056], in_=wsrc[:, :, 1024:2056]), writes=["w_in_b"])
        pg.dma("pool", lambda e: e.dma_start(out=wpool_b[:], in_=din["w_pool"][l].rearrange("g c d -> c g d")), writes=["wpool_b"])
        pg.dma("sp", lambda e: e.dma_start(out=pscol[:], in_=din["ps_col"][l]), writes=["pscol"])
        pg.dma("sp", lambda e: e.dma_start(out=bfg[:], in_=din["b_fgate"][l:l + 1, :].partition_broadcast(128)[:, 0, :]), writes=["bfg"])
        hT = [arenaA[:, i * 4096:(i + 1) * 4096].rearrange("p (k t) -> p k t", k=8) for i in range(2)]
        uring = arenaA[:, 8192:12288].rearrange("p (r n) -> p r n", r=8)
        vtile = [arenaA[:, 12288 + i * 512:12288 + (i + 1) * 512] for i in range(2)]
        qks = [arenaA[:, 13312 + i * 512:13312 + (i + 1) * 512] for i in range(2)]
        dft = [arenaA[:, 14336 + i * 512:14336 + (i + 1) * 512] for i in range(2)]
        def p1_L(i):
            xs_ = i % 3
            pg.dma("sp", lambda e: e.dma_start(out=xt[xs_][:], in_=xsrc[i * 128:(i + 1) * 128, :]),
                   reads=[("xres", i)], writes=[("xt", xs_)])

        def p1_A0(i):
            rms_sq(xt[i % 3][:], ("xt", i % 3), i)

        def p1_A1(i):
            rms_apply(xt[i % 3][:], ("xt", i % 3), i, G1, SH1, hb[i % 2][:], ("hb", i % 2), i % 2)

        def p1_A2(i):
            B, r = divmod(i, 4)
            pb, pk = ps_next()
            pTv = pb[:].bitcast(BF16).rearrange("p (k t) -> p k t", k=8)
            for k in range(8):
                pg.op("pe", lambda e: e.transpose(pTv[:, k, :], hb[i % 2][:, k * 128:(k + 1) * 128], ident_b[:]),
                      reads=[("hb", i % 2), "ident_b"], writes=[pk])
            pg.op("act", lambda e: e.activation(hT[B % 2][:, :, r * 128:(r + 1) * 128], pTv, AF.Copy),
                  reads=[pk], writes=[("hT", B % 2, r)])

        def p1_B(i):
            B, r = divmod(i, 4)
            pv, pvk = ps_next()
            pu, puk = ps_next()
            pf, pfk = ps_next()
            for (pp, ppk, c0, c1) in ((pv, pvk, 1024, 1536), (pu, puk, 1544, 2056), (pf, pfk, 1536, 1544)):
                for k in range(8):
                    pg.op("pe", lambda e: e.matmul(
                        pp[:, 0:c1 - c0], hT[B % 2][:, k, r * 128:(r + 1) * 128], w_in_b[:, k, c0:c1], start=(k == 0), stop=(k == 7)),
                        reads=[("hT", B % 2, r), "w_in_b"], writes=[ppk])
            vs = i % 2
            pg.op("dve", lambda e: e.tensor_copy(vtile[vs], pv[:]), reads=[pvk], writes=[("vtile", vs)])
            pg.dma("sp", lambda e: e.dma_start(out=v_d[i * 128:(i + 1) * 128, :], in_=vtile[vs]),
                   reads=[("vtile", vs)], writes=[("v_d", i)])
            us = i % 8
            pg.op("act", lambda e: e.activation(uring[:, us, :], pu[:], AF.Copy), reads=[puk], writes=[("uring", us)])
            pg.op("dve", lambda e: e.tensor_copy(zf[:, :, i], pf[:, 0:8]), reads=[pfk], writes=[("zf", i)])

        def p1_QK(B):
            for c in range(8):
                pq, pqk = ps_next()
                for k in range(8):
                    pg.op("pe", lambda e: e.matmul(pq[:], w_in_b[:, k, c * 128:(c + 1) * 128], hT[B % 2][:, k, :],
                                                   start=(k == 0), stop=(k == 7)),
                          reads=[("hT", B % 2, r_) for r_ in range(4)] + ["w_in_b"], writes=[pqk])
                qs = c % 2
                if c % 2 == 0:
                    pg.op("dve", lambda e: e.tensor_copy(qks[qs], pq[:]), reads=[pqk], writes=[("qks", qs)])
                else:
                    pg.op("act", lambda e: e.activation(qks[qs], pq[:], AF.Copy), reads=[pqk], writes=[("qks", qs)])
                dst = qT_d if c < 4 else kT_d
                h0 = (c % 4) * 2
                for hh in range(2):
                    pg.dma("sp", lambda e: e.dma_start(
                        out=dst[h0 + hh, 0:64, B * 512:(B + 1) * 512], in_=qks[qs][hh * 64:(hh + 1) * 64, :]),
                        reads=[("qks", qs)], writes=[("qkd", c, hh, B)])

        def p1_POOL(B):
            for g in range(4):
                w = POOL_W[g]
                pd, pdk = ps_next()
                for r in range(4):
                    i = B * 4 + r
                    bsel = g * 3 + (2 if i == 0 else 0)
                    pg.op("pe", lambda e: e.matmul(
                        pd[:, r * 128:(r + 1) * 128], uring[:, i % 8, g * 128:(g + 1) * 128], band_b[:, bsel, :], start=True, stop=(i == 0)),
                        reads=[("uring", i % 8), "band_b"], writes=[pdk])
                    if i > 0:
                        pg.op("pe", lambda e: e.matmul(
                            pd[:, r * 128:(r + 1) * 128], uring[:, (i - 1) % 8, g * 128:(g + 1) * 128], band_b[:, g * 3 + 1, :], start=False, stop=True),
                            reads=[("uring", (i - 1) % 8), "band_b"], writes=[pdk])
                ds = g % 2
                if B == 0:
                    pg.op("dve", lambda e: e.tensor_tensor(dft[ds][:, 0:128], pd[:, 0:128], invc_f[:, g, :], ALU.mult),
                          reads=[pdk, "invc_f"], writes=[("dft", ds)])
                    pg.op("act", lambda e: e.activation(dft[ds][:, 128:512], pd[:, 128:512], AF.Copy, scale=1.0 / w),
                          reads=[pdk], writes=[("dft", ds)])
                else:
                    pg.op("act", lambda e: e.activation(dft[ds], pd[:], AF.Copy, scale=1.0 / w),
                          reads=[pdk], writes=[("dft", ds)])
                pm, pmk = ps_next()
                pg.op("pe", lambda e: e.matmul(pm[:], wpool_b[:, g, :], dft[ds], start=True, stop=True),
                      reads=["wpool_b", ("dft", ds)], writes=[pmk])
                pg.op("act", lambda e: e.activation(catT[:, 4 + g, B * 512:(B + 1) * 512], pm[:], AF.Identity, scale=pscol[:, g:g + 1]),
                      reads=[pmk, "pscol"], writes=[("catT", 4 + g, B * 4 + r_) for r_ in range(4)])

        def p1_FG():
            zkeys = [("zf", i) for i in range(NT)]
            pg.op("dve", lambda e: e.tensor_tensor(zf[:], zf[:], bfg[:].unsqueeze(2).to_broadcast([128, 8, NT]), ALU.add),
                  reads=zkeys + ["bfg"], writes=["zfa"])
            pg.op("act", lambda e: e.activation(fA[:], zf[:], AF.Exp, scale=-1.0), reads=["zfa"], writes=["fA"])
            pg.op("act", lambda e: e.activation(fB[:], fA[:], AF.Ln, bias=1.0), reads=["fA"], writes=["fB"])
            pl, plk = ps_next()
            pt2, ptk = ps_next()
            nlf2 = fB[:].rearrange("p h t -> p (h t)")
            pg.op("pe", lambda e: e.matmul(pl[:, 0:256], triu_f[:], nlf2, start=True, stop=True), reads=["fB", "triu_f"], writes=[plk])
            pg.op("pe", lambda e: e.matmul(pt2[:, 0:256], ones_f[:], nlf2, start=True, stop=True), reads=["fB", "ones_f"], writes=[ptk])
            pg.op("dve", lambda e: e.tensor_copy(ftot[:].rearrange("p h t -> p (h t)"), pt2[:, 0:256]), reads=[ptk], writes=["ftot"])
            pg.op("dve", lambda e: e.tensor_copy(fA[:], ftot[:]), reads=["ftot", "fA"], writes=["fA"])
            cur, oth, ck, ok_ = fA, fB, "fA", "fB"
            for sft in (1, 2, 4, 8, 16):
                pg.op("dve", lambda e, cur=cur, oth=oth, sft=sft: e.tensor_copy(oth[:, :, 0:sft], cur[:, :, 0:sft]),
                      reads=[ck, plk, ptk], writes=[ok_])
                pg.op("dve", lambda e, cur=cur, oth=oth, sft=sft: e.tensor_tensor(oth[:, :, sft:NT], cur[:, :, sft:NT], cur[:, :, 0:NT - sft], ALU.add),
                      reads=[ck], writes=[ok_])
                cur, oth, ck, ok_ = oth, cur, ok_, ck
            pg.op("dve", lambda e, cur=cur: e.tensor_tensor(Gm[:], cur[:], ftot[:], ALU.subtract), reads=[ck, "ftot"], writes=["Gm"])
            pg.op("dve", lambda e: e.tensor_tensor(Gm[:].rearrange("p h t -> p (h t)"), Gm[:].rearrange("p h t -> p (h t)"), pl[:, 0:256], ALU.add),
                  reads=["Gm", plk], writes=["Gm"])
            if "dbg_G" in dbg_d and l == 0:
                pg.dma("sp", lambda e: e.dma_start(out=dbg_d["dbg_G"], in_=Gm[:]), reads=["Gm"], writes=["dbgG"])
            for half in range(2):
                pt3, pt3k = ps_next()
                pg.op("pe", lambda e, pt3=pt3, half=half: e.transpose(pt3[:, 0:128], Gm[:, half * 4:(half + 1) * 4, :].rearrange("p h t -> p (h t)"), ident_f[:]),
                      reads=["Gm", "ident_f"], writes=[pt3k])
                pg.op("act", lambda e, pt3=pt3, half=half: e.activation(gtn[half][:], pt3[:, 0:128], AF.Copy, scale=-1.0),
                      reads=[pt3k], writes=[("gtn", half)])
                for hl in range(4):
                    h = half * 4 + hl
                    pg.dma("sp", lambda e, h=h, hl=hl, half=half: e.dma_start(
                        out=qT_d[h, 64, :].rearrange("(i t) -> i t", t=128), in_=gtn[half][hl * 32:(hl + 1) * 32, :]),
                        reads=[("gtn", half)], writes=[("qaug", h)])

        for it in range(-3, NT + 1):
            if 0 <= it + 3 < NT:
                p1_L(it + 3)
            if 0 <= it + 2 < NT:
                p1_A0(it + 2)
            if 0 <= it + 1 < NT:
                p1_A1(it + 1)
            if 0 <= it < NT:
                p1_A2(it)
            if 0 <= it - 1 < NT:
                p1_B(it - 1)
                if it - 1 == NT - 1:
                    p1_FG()
                if (it - 1) % 4 == 3:
                    p1_QK((it - 1) // 4)
                    p1_POOL((it - 1) // 4)
        if stop == "p1":
            if "dbg_cat" in dbg_d:
                pg.dma("sp", lambda e: e.dma_start(out=dbg_d["dbg_cat"], in_=catT), reads=[("catT", c, i) for c in range(8) for i in range(NT)], writes=["dbgcat"])
            break

        pg.fence(fsc[:, 1:2])
        qkh = arenaB[:].rearrange("p (s n) -> p s n", s=4)
        PT = [hb[0][:, 0:512], hb[0][:, 512:1024], hb[1][:, 0:512], hb[1][:, 512:1024]]
        po = [psb[0], psb[1], psb[2]]
        NSR = 4
        s_ring = [psb[3 + i] for i in range(NSR)]
        do_m = (l + 1 < nl)
        LA = 2
        all_qk_keys = [("qkd", c, hh, B) for c in range(8) for hh in range(2) for B in range(8)] + [("qaug", h) for h in range(8)] + ["kaug"]

        def load_head(h):
            sl = h % 2
            vs = h % 2
            pg.dma("sp", lambda e: e.dma_start(out=qkh[0:65, sl * 2 + 0, 0:S], in_=qT_d[h]), reads=all_qk_keys, writes=[("qh", sl)])
            pg.dma("sp", lambda e: e.dma_start(out=qkh[0:65, sl * 2 + 1, 0:S], in_=kT_d[h]), reads=all_qk_keys, writes=[("kh", sl)])
            off = 64 if h % 2 else 0
            pg.dma("sp", lambda e: e.dma_start(out=vaug[vs][:, :, off:off + 64], in_=v_d.rearrange("(i p) c -> p i c", p=128)[:, :, h * 64:(h + 1) * 64]),
                   reads=[("v_d", i) for i in range(NT)], writes=[("vaug", vs)])

        tiles = []
        blk = 0
        for h in range(NH):
            for I in range(8):
                n_kt = 4 * (I + 1)
                for j in range(n_kt - 1, -1, -1):
                    tiles.append((h, I, j, blk, j == n_kt - 1, j == 0))
                blk += 1

        def emit_S(n):
            h, I, j, blk_, first, last = tiles[n]
            sl = h % 2
            qh = qkh[0:65, sl * 2 + 0, 0:S]
            kh = qkh[0:65, sl * 2 + 1, 0:S]
            r = j - 4 * I
            q0 = max(r, 0) * 128
            sb_ = s_ring[n % NSR]
            sk = ("ps", 3 + n % NSR)
            kt = kh[:, j * 128:(j + 1) * 128]
            rd = [("qh", sl), ("kh", sl)]
            if r >= 0:
                pg.op("pe", lambda e: e.matmul(sb_[:, q0:q0 + 128], kt, qh[:, I * 512 + q0:I * 512 + q0 + 128], start=True, stop=False),
                      reads=rd, writes=[sk])
                pg.op("pe", lambda e: e.matmul(sb_[:, q0:q0 + 128], negm_b[:], ident_b[:], start=False, stop=True),
                      reads=["negm_b", "ident_b"], writes=[sk])
                if q0 + 128 < 512:
                    pg.op("pe", lambda e: e.matmul(sb_[:, q0 + 128:512], kt, qh[:, I * 512 + q0 + 128:(I + 1) * 512], start=True, stop=True),
                          reads=rd, writes=[sk])
            else:
                pg.op("pe", lambda e: e.matmul(sb_[:], kt, qh[:, I * 512:(I + 1) * 512], start=True, stop=True), reads=rd, writes=[sk])

        def emit_exp_pv(n):
            h, I, j, blk_, first, last = tiles[n]
            sl = h % 2
            vs = h % 2
            qh = qkh[0:65, sl * 2 + 0, 0:S]
            r = j - 4 * I
            q0 = max(r, 0) * 128
            sb_ = s_ring[n % NSR]
            sk = ("ps", 3 + n % NSR)
            ptl = PT[n % 4]
            ptk_ = ("PT", n % 4)
            pob = po[blk_ % 3]
            pok = ("ps", blk_ % 3)
            if first:
                pg.op("pe", lambda e: e.matmul(pob[:], zero_b[0:65, :], qh[:, I * 512:(I + 1) * 512], start=True, stop=False),
                      reads=["zero_b", ("qh", sl)], writes=[pok])
            pg.op("act", lambda e: e.activation(ptl[:, q0:512], sb_[:, q0:512], AF.Exp, bias=Gm[:, h, j:j + 1], scale=0.125),
                  reads=[sk, "Gm"], writes=[ptk_])
            pg.op("pe", lambda e: e.matmul(pob[:, q0:512], vaug[vs][:, j, :], ptl[:, q0:512], start=False, stop=last),
                  reads=[("vaug", vs), ptk_], writes=[pok])

        def emit_norm(h, I, blk_):
            odd = h % 2
            rows = slice(64, 128) if odd else slice(0, 64)
            drows = slice(0, 64) if odd else slice(64, 128)
            pob = po[blk_ % 3]
            rr = tmpf[(blk_ % 3) // 2][:, ((blk_ % 3) % 2) * 512:((blk_ % 3) % 2 + 1) * 512]
            pg.op("dve", lambda e: e.reciprocal(rr[drows, :], pob[drows, :]), reads=[("ps", blk_ % 3)], writes=[("rrow", blk_ % 3)])
            pg.op("dve", lambda e: e.tensor_tensor(catT[rows, h // 2, I * 512:(I + 1) * 512], pob[rows, :], rr[drows, :], ALU.mult),
                  reads=[("ps", blk_ % 3), ("rrow", blk_ % 3)], writes=[("catT", h // 2, I * 4 + r_, odd) for r_ in range(4)])

        load_head(0)
        load_head(1)
        ntl = len(tiles)
        for n in range(min(LA, ntl)):
            emit_S(n)
        for n in range(ntl):
            h, I, j, blk_, first, last = tiles[n]
            if n + LA < ntl:
                emit_S(n + LA)
            if first and (blk_ % 2 == 0):
                conv_step(3)
            if first and (blk_ % 2 == 1) and do_m:
                ev = blk_ // 2
                if ev == 0:
                    m_load(l + 1, 0)
                    m_load(l + 1, 1)
                if ev < NMC:
                    m_comp(ev, 7)
                    if ev + 2 < NMC:
                        m_load(l + 1, ev + 2)
            emit_exp_pv(n)
            if last:
                emit_norm(h, I, blk_)
                if I == 7 and h + 2 < NH:
                    load_head(h + 2)
        if stop == "p2":
            if "dbg_cat" in dbg_d:
                pg.dma("sp", lambda e: e.dma_start(out=dbg_d["dbg_cat"], in_=catT),
                       reads=[("catT", c, i) for c in range(4, 8) for i in range(NT)] + [("catT", c, i, o_) for c in range(4) for i in range(NT) for o_ in range(2)],
                       writes=["dbgcat"])
            break

        pg.fence(fsc[:, 2:3])
        w_out_b = arenaB[:, 0:8 * D].rearrange("p (k n) -> p k n", k=8)
        pg.dma("pool", lambda e: e.dma_start(out=w_out_b, in_=din["w_out"][l].rearrange("(k p) n -> p k n", p=128)), writes=["w_out_b"])
        pg.dma("pool", lambda e: e.dma_start(out=wr_b[:], in_=din["w_r"][l].rearrange("(k p) n -> p k n", p=128)), writes=["wr_b"])
        pg.dma("sp", lambda e: e.dma_start(out=brb[:], in_=din["b_r"][l:l + 1, :].partition_broadcast(128)[:, 0, :]), writes=["brb"])
        hT2 = [arenaB[:, 8192 + i * 1024:8192 + (i + 1) * 1024].rearrange("p (k t) -> p k t", k=8) for i in range(2)]
        p3_pm = {}

        def p3_M(i):
            xs_ = i % 3
            pg.dma("sp", lambda e: e.dma_start(out=xt[xs_][:], in_=xsrc[i * 128:(i + 1) * 128, :]), reads=[("xres", i)], writes=[("xt", xs_)])
            cat_keys = [("catT", c, i) for c in range(4, 8)] + [("catT", c, i, o_) for c in range(4) for o_ in range(2)]
            pmx = []
            for half in range(2):
                bnk = (i % 3) * 2 + half
                pm_, pmk_ = psb[bnk], ("ps", bnk)
                pmx.append((pm_, pmk_))
                for c in range(8):
                    pg.op("pe", lambda e: e.matmul(
                        pm_[:], catT[:, c, i * 128:(i + 1) * 128], w_out_b[:, c, half * 512:(half + 1) * 512], start=(c == 0), stop=(c == 7)),
                        reads=cat_keys + ["w_out_b"], writes=[pmk_])
            p3_pm[i] = pmx

        def p3_R1(i):
            xs_ = i % 3
            ts = i % 2
            pmx = p3_pm.pop(i)
            for half in range(2):
                pm_, pmk_ = pmx[half]
                pg.op("dve", lambda e: e.tensor_tensor(
                    tmpf[ts][:, half * 512:(half + 1) * 512], pm_[:], GTM[:, half * 512:(half + 1) * 512], ALU.mult),
                    reads=[pmk_, "mod"], writes=[("tmpf", ts)])
            xn_ = i % 2
            pg.op("dve", lambda e: e.tensor_tensor(xnt[xn_][:], tmpf[ts][:], xt[xs_][:], ALU.add),
                  reads=[("tmpf", ts), ("xt", xs_)], writes=[("xnt", xn_)])
            pg.dma("sp", lambda e: e.dma_start(out=xres[i * 128:(i + 1) * 128, :], in_=xnt[xn_][:]),
                   reads=[("xnt", xn_)], writes=[("xres", i)])
            rms_sq(xnt[xn_][:], ("xnt", xn_), i)

        def p3_R2(i):
            xn_ = i % 2
            rms_apply(xnt[xn_][:], ("xnt", xn_), i, G2, SH2, hb[i % 2][:], ("hb", i % 2), i % 2)
            pg.dma("sp", lambda e: e.dma_start(out=h_d[i * 128:(i + 1) * 128, :], in_=hb[i % 2][:]),
                   reads=[("hb", i % 2)], writes=[("h_d", i)])

        def p3_R3(i):
            pb, pk = psb[6], ("ps", 6)
            pTv = pb[:].bitcast(BF16).rearrange("p (k t) -> p k t", k=8)
            for k in range(8):
                pg.op("pe", lambda e: e.transpose(pTv[:, k, :], hb[i % 2][:, k * 128:(k + 1) * 128], ident_b[:]),
                      reads=[("hb", i % 2), "ident_b"], writes=[pk])
            pg.op("act", lambda e: e.activation(hT2[i % 2], pTv, AF.Copy), reads=[pk], writes=[("hT2", i % 2)])

        def p3_LG(i):
            plg, plgk = psb[7], ("ps", 7)
            for k in range(8):
                pg.op("pe", lambda e: e.matmul(plg[:, 0:36], hT2[i % 2][:, k, :], wr_b[:, k, :], start=(k == 0), stop=(k == 7)),
                      reads=[("hT2", i % 2), "wr_b"], writes=[plgk])
            pg.op("dve", lambda e: e.tensor_tensor(LG[:, i, :], plg[:, 0:36], brb[:], ALU.add),
                  reads=[plgk, "brb"], writes=[("LG", i)])

        for i in range(min(3, NT)):
            p3_M(i)
        for it in range(NT + 3):
            if it < NT:
                p3_R1(it)
            if it + 3 < NT:
                p3_M(it + 3)
            if 0 <= it - 1 < NT:
                p3_R2(it - 1)
            if 0 <= it - 2 < NT:
                p3_R3(it - 2)
            if 0 <= it - 3 < NT:
                p3_LG(it - 3)
        if "dbg_LG" in dbg_d and l == 0:
            pg.dma("sp", lambda e: e.dma_start(out=dbg_d["dbg_LG"], in_=LG[:]), reads=[("LG", i) for i in range(NT)], writes=["dbgLG"])

        pg.fence(fsc[:, 3:4])
        RA = arenaA[:].bitcast(F32)

        def rt(idx_, n=1024):
            return RA[:, idx_ * 1024:idx_ * 1024 + n]
        vm = rt(0).rearrange("p (i g) -> p i g", i=NT)
        vm4 = rt(0).rearrange("p (i g e) -> p i g e", i=NT, g=4)
        oh1 = rt(1).rearrange("p (i g) -> p i g", i=NT)
        oh2 = rt(2).rearrange("p (i g) -> p i g", i=NT)
        vm2 = rt(3).rearrange("p (i g) -> p i g", i=NT)
        tA = rt(4).rearrange("p (i g) -> p i g", i=NT)
        tB = rt(5).rearrange("p (i g) -> p i g", i=NT)
        rin = rt(6).rearrange("p (i g) -> p i g", i=NT)
        tot = rt(7).rearrange("p (i g) -> p i g", i=NT)
        cmp3 = RA[:, 8 * 1024:8 * 1024 + NTILE * 32].rearrange("p (j e) -> p j e", e=32)
        sm = RA[:, 11 * 1024:12 * 1024]
        ohb = arenaA[:, 12 * 2048:12 * 2048 + 1024]
        gmax, sumg, topp = sm[:, 0:32], sm[:, 32:64], sm[:, 64:96]
        m1, m2, e2 = sm[:, 96:128], sm[:, 128:160], sm[:, 160:192]
        ew1, pos1f, pos2f = sm[:, 192:224], sm[:, 224:256], sm[:, 256:288]
        cntv, pcv, t1v = sm[:, 288:320], sm[:, 320:352], sm[:, 352:384]
        offA, offB, endo = sm[:, 384:416], sm[:, 416:448], sm[:, 448:480]
        ohg = sm[:, 480:608].rearrange("p (i g) -> p i g", g=4)
        egx = sm[:, 608:736].rearrange("p (i g) -> p i g", g=4)
        pen = sm[:, 736:864].rearrange("p (i g) -> p i g", g=4)
        tef = sm[:, 864:960]
        LGg = LG[:, :, 0:4]
        LE4 = LG[:, :, 4:36].rearrange("p i (g e) -> p i g e", g=4)
        RK = "route"
        lgk = [("LG", i) for i in range(NT)]
        NSL = 8
        hsl = [arenaB[:, i * 1024:(i + 1) * 1024] for i in range(NSL)]

        def hsl_load(i):
            pg.dma("sp", lambda e: e.dma_start(out=hsl[i % NSL], in_=h_d[i * 128:(i + 1) * 128, :]),
                   reads=[("h_d", i)], writes=[("hsl", i % NSL)])
        for i in range(NSL):
            hsl_load(i)

        def dv(fn, reads=(), eng="dve"):
            pg.op(eng, fn, reads=[RK] + list(reads), writes=[RK])
        dv(lambda e: e.tensor_reduce(gmax, LGg, AX.X, ALU.max), reads=lgk)
        dv(lambda e: e.tensor_tensor(ohg, LGg, gmax.unsqueeze(2).to_broadcast([128, NT, 4]), ALU.is_equal))
        dv(lambda e: e.tensor_tensor(egx, LGg, gmax.unsqueeze(2).to_broadcast([128, NT, 4]), ALU.subtract))
        dv(lambda e: e.activation(egx, egx, AF.Exp), eng="act")
        dv(lambda e: e.tensor_reduce(sumg, egx, AX.X, ALU.add))
        dv(lambda e: e.reciprocal(topp, sumg))
        dv(lambda e: e.tensor_scalar(pen, ohg, BIG, -BIG, ALU.mult, ALU.add))
        dv(lambda e: e.tensor_tensor(vm4, LE4, pen.unsqueeze(3).to_broadcast([128, NT, 4, 8]), ALU.add))
        dv(lambda e: e.tensor_reduce(m1, vm, AX.X, ALU.max))
        dv(lambda e: e.tensor_tensor(oh1, vm, m1.unsqueeze(2).to_broadcast([128, NT, 32]), ALU.is_equal))
        dv(lambda e: e.scalar_tensor_tensor(vm2, oh1, -BIG, vm, ALU.mult, ALU.add))
        dv(lambda e: e.tensor_reduce(m2, vm2, AX.X, ALU.max))
        dv(lambda e: e.tensor_tensor(oh2, vm2, m2.unsqueeze(2).to_broadcast([128, NT, 32]), ALU.is_equal))
        dv(lambda e: e.tensor_tensor(e2, m2, m1, ALU.subtract))
        dv(lambda e: e.activation(e2, e2, AF.Exp), eng="act")
        dv(lambda e: e.tensor_scalar(ew1, e2, 1.0, None, ALU.add))
        dv(lambda e: e.reciprocal(ew1, ew1))
        dv(lambda e: e.tensor_tensor(comb[0][:], topp, ew1, ALU.mult))
        dv(lambda e: e.tensor_tensor(comb[1][:], comb[0][:], e2, ALU.mult))
        dv(lambda e: e.tensor_tensor(ohb.rearrange("p (i g) -> p i g", i=NT), oh1, oh2, ALU.add))
        for (dst, lhs, lk) in ((rin, ltri_b, "ltri_b"), (tot, ones_b, "ones_b")):
            for half in range(2):
                pb, pk = ps_next()
                pg.op("pe", lambda e, pb=pb, lhs=lhs, half=half: e.matmul(pb[:], lhs[:], ohb[:, half * 512:(half + 1) * 512], start=True, stop=True),
                      reads=[RK, lk], writes=[pk])
                pg.op("dve", lambda e, pb=pb, dst=dst, half=half: e.tensor_copy(dst[:, half * 16:(half + 1) * 16, :].rearrange("p i g -> p (i g)"), pb[:]),
                      reads=[pk, RK], writes=[RK])
        dv(lambda e: e.tensor_copy(tA, tot))
        cur, oth = tA, tB
        for sft in (1, 2, 4, 8, 16):
            dv(lambda e, cur=cur, oth=oth, sft=sft: e.tensor_copy(oth[:, 0:sft, :], cur[:, 0:sft, :]))
            dv(lambda e, cur=cur, oth=oth, sft=sft: e.tensor_tensor(oth[:, sft:NT, :], cur[:, sft:NT, :], cur[:, 0:NT - sft, :], ALU.add))
            cur, oth = oth, cur
        inc3 = cur
        dv(lambda e: e.tensor_copy(cntv, inc3[:, NT - 1, :]))
        dv(lambda e: e.tensor_scalar(t1v, cntv, 127.0, None, ALU.add))
        dv(lambda e: e.tensor_scalar(pcv, t1v, 1.0 / 128, -0.49609375, ALU.mult, ALU.add))
        dv(lambda e: e.tensor_scalar(pcv, pcv, 8388608.0, None, ALU.add))
        dv(lambda e: e.tensor_scalar(pcv, pcv, -8388608.0, 128.0, ALU.add, ALU.mult))
        dv(lambda e: e.tensor_copy(offA, pcv))
        c2, o2 = offA, offB
        for sft in (1, 2, 4, 8, 16):
            dv(lambda e, c2=c2, o2=o2, sft=sft: e.tensor_copy(o2[:, 0:sft], c2[:, 0:sft]))
            dv(lambda e, c2=c2, o2=o2, sft=sft: e.tensor_tensor(o2[:, sft:32], c2[:, sft:32], c2[:, 0:32 - sft], ALU.add))
            c2, o2 = o2, c2
        dv(lambda e, c2=c2: e.tensor_copy(endo, c2))
        dv(lambda e: e.tensor_tensor(offA if c2 is not offA else offB, endo, pcv, ALU.subtract))
        offx = offA if c2 is not offA else offB
        dv(lambda e: e.tensor_tensor(oth, inc3, tot, ALU.subtract))
        dv(lambda e: e.tensor_tensor(oth, oth, rin, ALU.add))
        dv(lambda e: e.tensor_tensor(oth, oth, offx.unsqueeze(1).to_broadcast([128, NT, 32]), ALU.add))
        dv(lambda e: e.tensor_tensor(vm, oth, oh1, ALU.mult))
        dv(lambda e: e.tensor_reduce(pos1f, vm, AX.X, ALU.add))
        dv(lambda e: e.tensor_tensor(vm, oth, oh2, ALU.mult))
        dv(lambda e: e.tensor_reduce(pos2f, vm, AX.X, ALU.add))
        dv(lambda e: e.tensor_copy(pos_i[0][:], pos1f))
        dv(lambda e: e.tensor_copy(pos_i[1][:], pos2f))
        dv(lambda e: e.tensor_tensor(cmp3, endo.unsqueeze(1).to_broadcast([128, NTILE, 32]), jv_f[:].unsqueeze(2).to_broadcast([128, NTILE, 32]), ALU.is_le),
           reads=["jv_f"])
        dv(lambda e: e.tensor_reduce(tef, cmp3, AX.X, ALU.add))
        dv(lambda e: e.tensor_scalar(tef, tef, 128.0, None, ALU.mult))
        dv(lambda e: e.tensor_scalar(tef, tef, pidx_f[:, 0:1], None, ALU.add), reads=["pidx_f"])
        dv(lambda e: e.tensor_copy(idxW[:], tef))
        if "dbg_route" in dbg_d and l == 0:
            dr = dbg_d["dbg_route"]
            for n_, src_ in enumerate((pos1f, pos2f, comb[0][:], comb[1][:], m1, m2)):
                pg.dma("sp", lambda e, n_=n_, src_=src_: e.dma_start(out=dr[:, n_, :], in_=src_), reads=[RK], writes=[("dbgr", n_)])
            if "dbg_te" in dbg_d:
                pg.dma("sp", lambda e: e.dma_start(out=dbg_d["dbg_te"], in_=tef), reads=[RK], writes=["dbgte"])
        for i in range(NT):
            for k in range(2):
                pg.dma("pool", lambda e: e.indirect_dma_start(
                    out=hs_d, out_offset=bass.IndirectOffsetOnAxis(ap=pos_i[k][:, i:i + 1], axis=0), in_=hsl[i % NSL], in_offset=None),
                    reads=[("hsl", i % NSL), RK], writes=[("hs_d", i, k)])
            if i + NSL < NT:
                hsl_load(i + NSL)
        if stop == "p3":
            break

        pg.fence(fsc[:, 4:5])
        NW = 5
        Wt = [arenaA[:, i * 6144:(i + 1) * 6144] for i in range(NW)]
        o4 = 0
        hst = [arenaB[:, o4 + i * 1024:o4 + (i + 1) * 1024] for i in range(3)]
        o4 += 3 * 1024
        hsT = [arenaB[:, o4 + i * 1024:o4 + (i + 1) * 1024].rearrange("p (k t) -> p k t", k=8) for i in range(3)]
        o4 += 3 * 1024
        sa = [arenaB[:, o4 + i * 512:o4 + (i + 1) * 512].bitcast(F32) for i in range(3)]
        o4 += 3 * 512
        actb = [arenaB[:, o4 + i * 256:o4 + (i + 1) * 256] for i in range(3)]
        o4 += 3 * 256
        actT = [arenaB[:, o4 + i * 256:o4 + (i + 1) * 256].rearrange("p (k t) -> p k t", k=2) for i in range(3)]
        o4 += 3 * 256
        yt = [xnt[0], xnt[1]]
        hs_keys = [("hs_d", i, k) for i in range(NT) for k in range(2)]
        wbf_keys = [("wbf", l, e_, p_) for e_ in range(32) for p_ in range(3)]
        while conv_items and conv_items[0][0] <= l:
            conv_step(1)

        def moe_load(j):
            pg.dma("pool", lambda e: e.indirect_dma_start(
                out=Wt[j % NW], out_offset=None, in_=wbf_d[l], in_offset=bass.IndirectOffsetOnAxis(ap=idxW[:, j:j + 1], axis=0),
                bounds_check=pg.wbound, oob_is_err=False),
                reads=wbf_keys + [RK], writes=[("Wt", j % NW)])
            pg.dma("sp", lambda e: e.dma_start(out=hst[j % 3], in_=hs_d[j * 128:(j + 1) * 128, :]),
                   reads=hs_keys, writes=[("hst", j % 3)])

        def moe_S1(j):
            pb, pk = ps_next()
            pTv = pb[:].bitcast(BF16).rearrange("p (k t) -> p k t", k=8)
            for k in range(8):
                pg.op("pe", lambda e: e.transpose(pTv[:, k, :], hst[j % 3][:, k * 128:(k + 1) * 128], ident_b[:]),
                      reads=[("hst", j % 3), "ident_b"], writes=[pk])
            pg.op("act", lambda e: e.activation(hsT[j % 3], pTv, AF.Copy), reads=[pk], writes=[("hsT", j % 3)])

        def moe_S2(j):
            w_ = Wt[j % NW]
            pgu, pguk = ps_next()
            for k in range(8):
                pg.op("pe", lambda e: e.matmul(pgu[:], hsT[j % 3][:, k, :], w_[:, k * 512:(k + 1) * 512], start=(k == 0), stop=(k == 7)),
                      reads=[("hsT", j % 3), ("Wt", j % NW)], writes=[pguk])
            pg.op("act", lambda e: e.activation(sa[j % 3], pgu[:, 0:256], AF.Silu), reads=[pguk], writes=[("sa", j % 3)])
            pg.op("dve", lambda e: e.tensor_tensor(actb[j % 3], sa[j % 3], pgu[:, 256:512], ALU.mult),
                  reads=[pguk, ("sa", j % 3)], writes=[("actb", j % 3)])

        def moe_S3(j):
            pb2, pk2 = ps_next()
            pT2 = pb2[:].bitcast(BF16)[:, 0:256].rearrange("p (k t) -> p k t", k=2)
            for k in range(2):
                pg.op("pe", lambda e: e.transpose(pT2[:, k, :], actb[j % 3][:, k * 128:(k + 1) * 128], ident_b[:]),
                      reads=[("actb", j % 3), "ident_b"], writes=[pk2])
            pg.op("dve", lambda e: e.tensor_copy(actT[j % 3], pT2), reads=[pk2], writes=[("actT", j % 3)])

        def moe_S4(j):
            w_ = Wt[j % NW]
            for half in range(2):
                py, pyk = ps_next()
                for k in range(2):
                    pg.op("pe", lambda e: e.matmul(
                        py[:], actT[j % 3][:, k, :], w_[:, 4096 + k * 1024 + half * 512:4096 + k * 1024 + (half + 1) * 512], start=(k == 0), stop=(k == 1)),
                        reads=[("actT", j % 3), ("Wt", j % NW)], writes=[pyk])
                pg.op("dve", lambda e: e.tensor_tensor(yt[j % 2][:, half * 512:(half + 1) * 512], py[:], GTF[:, half * 512:(half + 1) * 512], ALU.mult),
                      reads=[pyk, "mod"], writes=[("yt", j % 2, half)])
            pg.dma("sp", lambda e: e.dma_start(out=ys_d[j * 128:(j + 1) * 128, :], in_=yt[j % 2][:]),
                   reads=[("yt", j % 2, 0), ("yt", j % 2, 1)], writes=[("ys_d", j)])

        PF = 2
        for j in range(min(PF, NTILE)):
            moe_load(j)
        for s_ in range(NTILE + 3):
            if 0 <= s_ - 3 < NTILE:
                moe_S4(s_ - 3)
            if s_ + PF < NTILE:
                moe_load(s_ + PF)
            if s_ < NTILE:
                moe_S1(s_)
            if 0 <= s_ - 1 < NTILE:
                moe_S2(s_ - 1)
            if 0 <= s_ - 2 < NTILE:
                moe_S3(s_ - 2)
        if stop == "p4":
            break

        pg.fence(fsc[:, 5:6])
        ys_keys = [("ys_d", j) for j in range(NTILE)]
        y12 = [[arenaA[:, (2 * s_ + k) * 2048:(2 * s_ + k + 1) * 2048].bitcast(F32) for k in range(2)] for s_ in range(3)]
        last = (l == nl - 1)
        if last:
            pg.dma("sp", lambda e: e.dma_start(out=gb, in_=din["norm_final_g"][0:1, :].partition_broadcast(128)[:, 0, :]), writes=["gb"])
        else:
            mod_reload(l + 1)

        def comb_load(i):
            for k in range(2):
                pg.dma("pool", lambda e, i=i, k=k: e.indirect_dma_start(
                    out=y12[i % 3][k], out_offset=None, in_=ys_d, in_offset=bass.IndirectOffsetOnAxis(ap=pos_i[k][:, i:i + 1], axis=0)),
                    reads=ys_keys + [RK], writes=[("y12", i % 3, k)])
            pg.dma("sp", lambda e, i=i: e.dma_start(out=xt[i % 3][:], in_=xres[i * 128:(i + 1) * 128, :]),
                   reads=[("xres", i)], writes=[("xt", i % 3)])

        comb_load(0)
        comb_load(1)
        for i in range(NT):
            if i + 2 < NT:
                comb_load(i + 2)
            ya, yb_ = y12[i % 3]
            ts = i % 2
            pg.op("act", lambda e, ya=ya, i=i, ts=ts: e.activation(tmpf[ts][:], ya, AF.Copy, scale=comb[0][:, i:i + 1]),
                  reads=[("y12", i % 3, 0), RK], writes=[("tmpf", ts)])
            pg.op("dve", lambda e, yb_=yb_, i=i, ts=ts: e.scalar_tensor_tensor(tmpf[ts][:], yb_, comb[1][:, i:i + 1], tmpf[ts][:], ALU.mult, ALU.add),
                  reads=[("y12", i % 3, 1), RK, ("tmpf", ts)], writes=[("tmpf", ts)])
            xn_ = i % 2
            pg.op("dve", lambda e, ts=ts, i=i, xn_=xn_: e.tensor_tensor(xnt[xn_][:], tmpf[ts][:], xt[i % 3][:], ALU.add),
                  reads=[("tmpf", ts), ("xt", i % 3)], writes=[("xnt", xn_)])
            if not last:
                pg.dma("sp", lambda e, xn_=xn_, i=i: e.dma_start(out=xres[i * 128:(i + 1) * 128, :], in_=xnt[xn_][:]),
                       reads=[("xnt", xn_)], writes=[("xres", i)])
            else:
                pg.op("act", lambda e, xn_=xn_, i=i: e.activation(sq[:], xnt[xn_][:], AF.Square, accum_out=ssq[:, i:i + 1]),
                      reads=[("xnt", xn_)], writes=["sq", ("ssq", i)])
                pg.op("dve", lambda e, i=i: e.tensor_scalar(rstd[:, i:i + 1], ssq[:, i:i + 1], 1.0 / D, EPS, ALU.mult, ALU.add),
                      reads=[("ssq", i)], writes=[("rstd", i)])
                pg.op("act", lambda e, i=i: e.activation(rstd[:, i:i + 1], rstd[:, i:i + 1], AF.Ln), reads=[("rstd", i)], writes=[("rstd", i)])
                pg.op("act", lambda e, i=i: e.activation(rstd[:, i:i + 1], rstd[:, i:i + 1], AF.Exp, scale=-0.5), reads=[("rstd", i)], writes=[("rstd", i)])
                pg.op("dve", lambda e, xn_=xn_, i=i: e.scalar_tensor_tensor(xnt[xn_][:], xnt[xn_][:], rstd[:, i:i + 1], gb, ALU.mult, ALU.mult),
                      reads=[("xnt", xn_), ("rstd", i), "gb"], writes=[("xnt", xn_)])
                pg.dma("sp", lambda e, xn_=xn_, i=i: e.dma_start(out=out_d[i * 128:(i + 1) * 128, :], in_=xnt[xn_][:]),
                       reads=[("xnt", xn_)], writes=[("out", i)])
    pg.emit()
    st.close()
    return nc, pg


def prep_inputs(inp):
    f = lambda a: np.ascontiguousarray(np.asarray(a, dtype=np.float32))
    shared = {}
    for k in ("norm_mix_g", "norm_ffn_g", "w_ada", "b_ada", "w_in", "b_fgate", "w_pool", "w_out"):
        shared[k] = f(inp[k])
    shared["norm_final_g"] = f(inp["norm_final_g"]).reshape(1, D)
    shared["ps_col"] = f(np.asarray(inp["pool_scale"]).reshape(NL, 4, 128).transpose(0, 2, 1))
    wre = np.asarray(inp["w_router_expert"]).transpose(0, 2, 1, 3).reshape(NL, D, 32)
    shared["w_r"] = f(np.concatenate([np.asarray(inp["w_router_group"]), wre], axis=2))
    shared["b_r"] = f(np.concatenate([np.asarray(inp["b_router_group"]), np.asarray(inp["b_router_expert"]).reshape(NL, 32)], axis=1))
    wg = np.asarray(inp["w_expert_gate"]).reshape(NL, 32, 8, 128, 256).transpose(0, 1, 3, 2, 4)
    wu = np.asarray(inp["w_expert_up"]).reshape(NL, 32, 8, 128, 256).transpose(0, 1, 3, 2, 4)
    wgu = np.concatenate([wg, wu], axis=4).reshape(NL, 32, 128, 4096)
    wd = np.asarray(inp["w_expert_down"]).reshape(NL, 32, 2, 128, 1024).transpose(0, 1, 3, 2, 4).reshape(NL, 32, 128, 2048)
    shared["wexp"] = f(np.concatenate([wgu, wd], axis=3).reshape(NL, 4096, 6144))
    for k, v in make_consts().items():
        shared["k_" + k] = f(v)
    x = np.asarray(inp["x"], dtype=np.float32)
    c = np.asarray(inp["c"], dtype=np.float32)
    per_core = []
    for b in range(8):
        m = dict(shared)
        m["x"] = np.ascontiguousarray(x[b])
        m["c_col"] = np.ascontiguousarray(c[b].reshape(8, 128).T)
        per_core.append(m)
    return per_core


_CACHE = {}


def kernel(**inputs):
    if "nc" not in _CACHE:
        _CACHE["nc"] = build()[0]
    nc = _CACHE["nc"]
    in_maps = prep_inputs(inputs)
    res = run_bass_kernel_spmd(nc, in_maps, core_ids=list(range(8)))
    return np.stack([np.asarray(r["out"], dtype=np.float32) for r in res.results], axis=0)
```

```python
import contextlib
import numpy as np
import concourse.bass as bass
import concourse.mybir as mybir
from concourse.bass_utils import run_bass_kernel_spmd

F32 = mybir.dt.float32
BF16 = mybir.dt.bfloat16
I32 = mybir.dt.int32
ALU = mybir.AluOpType
AF = mybir.ActivationFunctionType
AX = mybir.AxisListType

S = 4096
D = 1024
NT = 32
NL = 2
NH = 8
NTILE = 96
EPS = 1e-6
BIG = 30000.0
POOL_W = (2, 4, 8, 16)

COMPUTE = ("pe", "act", "dve", "pool")
DMAQ = ("sp", "pool", "act")


class _Rec:
    def __init__(self):
        self.call = None

    def __getattr__(self, name):
        def f(*a, **kw):
            assert self.call is None
            self.call = (name, a, kw)
            return self
        return f


class _Late:
    def __init__(self):
        self.v = None


def _replay(fn):
    rec = _Rec()
    fn(rec)
    name, a, kw = rec.call
    return lambda e: getattr(e, name)(*a, **{k: (v.v if isinstance(v, _Late) else v) for k, v in kw.items()})


class _Op:
    __slots__ = ("eng", "fn", "deps", "is_dma", "sig", "has_dep", "bg")

    def __init__(self, eng, fn, is_dma, bg):
        self.eng = eng
        self.fn = _replay(fn)
        self.deps = set()
        self.is_dma = is_dma
        self.sig = None
        self.has_dep = False
        self.bg = bg


class Prog:
    def __init__(self, nc, dma_ring=8):
        self.nc = nc
        self.ops = []
        self.last_writer = {}
        self.readers = {}
        self.dma_ring = dma_ring
        self.fence_op = None
        self.since_fence_dma = []
        self.last_eng_op = {}
        self.wbound = _Late()

    def _add(self, eng, fn, reads, writes, is_dma, bg):
        o = _Op(eng, fn, is_dma, bg)
        for k in reads:
            w = self.last_writer.get(k)
            if w is not None:
                o.deps.add(w)
        for k in writes:
            w = self.last_writer.get(k)
            if w is not None:
                o.deps.add(w)
            for r in self.readers.get(k, ()):
                o.deps.add(r)
        for k in writes:
            self.last_writer[k] = o
            self.readers[k] = []
        for k in reads:
            self.readers.setdefault(k, []).append(o)
        if not bg:
            if self.fence_op is not None:
                o.deps.add(self.fence_op)
            if is_dma:
                self.since_fence_dma.append(o)
            else:
                self.last_eng_op[eng] = o
        o.deps.discard(o)
        self.ops.append(o)
        return o

    def op(self, eng, fn, reads=(), writes=(), bg=False):
        return self._add(eng, fn, reads, writes, False, bg)

    def dma(self, eng, fn, reads=(), writes=(), bg=False):
        return self._add(eng, fn, reads, writes, True, bg)

    def fence(self, scratch):
        o = _Op("pool", lambda e: e.memset(scratch, 0.0), False, False)
        if self.fence_op is not None:
            o.deps.add(self.fence_op)
        for d in self.since_fence_dma:
            o.deps.add(d)
        for d in self.last_eng_op.values():
            o.deps.add(d)
        self.since_fence_dma = []
        self.last_eng_op = {"pool": o}
        self.fence_op = o
        self.ops.append(o)
        return o

    def emit(self):
        nc = self.nc
        ops = self.ops
        for o in ops:
            for d in o.deps:
                if d.is_dma:
                    d.has_dep = True
                elif d.eng == o.eng and d.eng == "pe" and not o.is_dma:
                    pass
                else:
                    d.has_dep = True
        stack = contextlib.ExitStack()
        sems = {e: stack.enter_context(nc.semaphore("s_" + e)) for e in COMPUTE}
        dsem = {q: [stack.enter_context(nc.semaphore("d_%s%d" % (q, i))) for i in range(self.dma_ring)]
                for q in DMAQ}
        cnt = {e: 0 for e in COMPUTE}
        dcnt = {q: [0] * self.dma_ring for q in DMAQ}
        dnum = {q: 0 for q in DMAQ}
        engs = ("pe", "act", "dve", "pool", "sp")
        waited = {e: {} for e in engs}
        streams = {e: [] for e in engs}
        for o in ops:
            eng = o.eng
            need = {}
            for d in o.deps:
                if d.sig is None:
                    continue
                if (not d.is_dma) and d.eng == eng and eng == "pe" and not o.is_dma:
                    continue
                s, v = d.sig
                if need.get(s, (None, 0))[1] < v:
                    need[s] = (s, v)
            if o.is_dma:
                r = dnum[eng] % self.dma_ring
                dnum[eng] += 1
                s = dsem[eng][r]
                prev = dcnt[eng][r]
                if prev > 0 and need.get(s, (None, 0))[1] < prev:
                    need[s] = (s, prev)
                dcnt[eng][r] += 16
                o.sig = (s, dcnt[eng][r])
                inc = (s, 16)
            elif o.has_dep:
                cnt[eng] += 1
                o.sig = (sems[eng], cnt[eng])
                inc = (sems[eng], 1)
            else:
                inc = None
            w = waited[eng]
            wl = []
            for s, v in need.values():
                if w.get(s, 0) >= v:
                    continue
                w[s] = v
                wl.append((s, v))
            streams[eng].append((wl, o.fn, inc))
        fin = []
        for q in DMAQ:
            for i in range(self.dma_ring):
                if dcnt[q][i] > 0:
                    fin.append((dsem[q][i], dcnt[q][i]))
        for e in COMPUTE:
            if cnt[e] > 0:
                fin.append((sems[e], cnt[e]))
        self.n_instr = {e: len(v) for e, v in streams.items()}

        def run(engname, e):
            if engname == "pool":
                r = e.alloc_register("wbound")
                e.reg_mov(r, 4095)
                self.wbound.v = r
            for wl, fn, inc in streams[engname]:
                for s, v in wl:
                    e.wait_ge(s, v)
                ins = fn(e)
                if inc is not None:
                    ins.then_inc(inc[0], inc[1])
            if engname == "sp":
                for s, v in fin:
                    e.wait_ge(s, v)

        with nc.Block() as block:
            @block.tensor
            def _(e):
                run("pe", e)

            @block.scalar
            def _(e):
                run("act", e)

            @block.vector
            def _(e):
                run("dve", e)

            @block.gpsimd
            def _(e):
                run("pool", e)

            @block.sync
            def _(e):
                run("sp", e)
        stack.close()


def make_consts():
    c = {}
    idx = np.arange(128)
    s_ = idx[:, None]
    t_ = idx[None, :]
    c["ident"] = np.eye(128, dtype=np.float32)
    c["triu"] = (s_ <= t_).astype(np.float32)
    c["ltri"] = (s_ < t_).astype(np.float32)
    c["ones"] = np.ones((128, 128), np.float32)
    c["negmask"] = np.where(t_ > s_, -BIG, 0.0).astype(np.float32)
    band = np.zeros((12, 128, 128), np.float32)
    invc = np.zeros((4, 128, 128), np.float32)
    for g, w in enumerate(POOL_W):
        same = ((s_ <= t_) & (s_ > t_ - w)).astype(np.float32)
        same[idx, idx] -= w
        prev = (s_ - 128 > t_ - w).astype(np.float32)
        first = ((s_ <= t_) & (s_ > t_ - w)).astype(np.float32)
        cntv = np.minimum(idx + 1, w).astype(np.float32)
        first[idx, idx] -= cntv
        band[g * 3 + 0] = same
        band[g * 3 + 1] = prev
        band[g * 3 + 2] = first
        invc[g] = np.broadcast_to((1.0 / cntv)[None, :], (128, 128))
    c["band"] = band
    c["invc"] = invc
    c["jv"] = np.broadcast_to((np.arange(NTILE, dtype=np.float32) * 128.0)[None, :], (128, NTILE)).copy()
    c["pidx"] = np.arange(128, dtype=np.float32)[:, None].copy()
    return c


CONST_SHAPES = {"ident": [128, 128], "triu": [128, 128], "ltri": [128, 128], "ones": [128, 128],
                "negmask": [128, 128], "band": [12, 128, 128], "invc": [4, 128, 128],
                "jv": [128, NTILE], "pidx": [128, 1]}

IN_SHAPES = {
    "x": [S, D], "c_col": [128, 8], "norm_mix_g": [NL, D], "norm_ffn_g": [NL, D], "norm_final_g": [1, D],
    "w_ada": [NL, D, 6 * D], "b_ada": [NL, 6 * D], "w_in": [NL, D, 2056], "b_fgate": [NL, 8],
    "w_pool": [NL, 4, 128, 128], "ps_col": [NL, 128, 4], "w_out": [NL, D, D],
    "w_r": [NL, D, 36], "b_r": [NL, 36], "wexp": [NL, 4096, 6144],
}


def build(nl=NL, stop=None, dbg=()):
    nc = bass.Bass("TRN2", target_bir_lowering=False)
    st = contextlib.ExitStack()
    din = {}
    need_in = set(IN_SHAPES)
    if stop in ("p1", "p2"):
        need_in -= {"w_out", "w_r", "b_r", "wexp", "norm_final_g"}
    if stop == "p3":
        need_in -= {"wexp", "norm_final_g"}
    for k in sorted(need_in):
        din[k] = nc.dram_tensor(k, IN_SHAPES[k], F32, kind="ExternalInput").ap()
    for k, shp in CONST_SHAPES.items():
        din["k_" + k] = nc.dram_tensor("k_" + k, shp, F32, kind="ExternalInput").ap()
    out_d = nc.dram_tensor("out", [S, D], F32, kind="ExternalOutput").ap()

    def scratch(name, shape, dt):
        kind = "ExternalOutput" if name in dbg else "Internal"
        return nc.dram_tensor(name, shape, dt, kind=kind).ap()

    xres = scratch("xres", [S, D], F32)
    qT_d = scratch("qT_d", [NH, 65, S], BF16)
    kT_d = scratch("kT_d", [NH, 65, S], BF16)
    v_d = scratch("v_d", [S, 512], BF16)
    h_d = scratch("h_d", [S, D], BF16)
    hs_d = scratch("hs_d", [NTILE * 128, D], BF16)
    ys_d = scratch("ys_d", [NTILE * 128, D], F32)
    wbf_d = [scratch("wbf%d" % l, [4096, 6144], BF16) for l in range(nl)]
    dbg_d = {}
    for name, shp, dt in (("dbg_cat", [128, 8, S], BF16), ("dbg_G", [128, 8, NT], F32),
                          ("dbg_mod", [128, 6 * D], F32), ("dbg_LG", [128, NT, 36], F32),
                          ("dbg_route", [128, 6, NT], F32), ("dbg_te", [128, NTILE], F32)):
        if name in dbg:
            dbg_d[name] = nc.dram_tensor(name, shp, dt, kind="ExternalOutput").ap()

    def T(name, shape, dt):
        return st.enter_context(nc.sbuf_tensor(name, shape, dt))

    pg = Prog(nc)
    psb = [st.enter_context(nc.psum_tensor("psb%d" % i, [128, 512], F32)) for i in range(8)]
    ps_rr = [0]

    def ps_next():
        b = ps_rr[0] % 8
        ps_rr[0] += 1
        return psb[b], ("ps", b)

    arenaA = T("arenaA", [128, 8 * S], BF16)
    arenaB = T("arenaB", [128, 8 * 2056], BF16)
    mod = T("mod", [128, 6 * D], F32)
    vaug = [T("vaug%d" % i, [128, NT, 128], BF16) for i in range(2)]
    stage = [T("stage%d" % i, [128, 2048], BF16) for i in range(2)]
    xt = [T("xt%d" % i, [128, D], F32) for i in range(3)]
    tmpf = [T("tmpf%d" % i, [128, D], F32) for i in range(2)]
    xnt = [T("xnt%d" % i, [128, D], F32) for i in range(2)]
    hb = [T("hb%d" % i, [128, D], BF16) for i in range(2)]
    sq = T("sq", [128, D], BF16)
    fsc = T("fsc", [128, 8], F32)
    ident_f = T("ident_f", [128, 128], F32)
    triu_f = T("triu_f", [128, 128], F32)
    ones_f = T("ones_f", [128, 128], F32)
    ident_b = T("ident_b", [128, 128], BF16)
    ltri_b = T("ltri_b", [128, 128], BF16)
    ones_b = T("ones_b", [128, 128], BF16)
    negm_b = T("negm_b", [128, 128], BF16)
    band_b = T("band_b", [128, 12, 128], BF16)
    invc_f = T("invc_f", [128, 4, 128], F32)
    jv_f = T("jv_f", [128, NTILE], F32)
    pidx_f = T("pidx_f", [128, 1], F32)
    zero_b = T("zero_b", [128, 128], BF16)
    epsc = T("epsc", [128, 1], F32)
    cb = T("cb", [128, 8, 128], BF16)
    ccol = T("ccol", [128, 8], F32)
    cact = T("cact", [128, 8], F32)
    ssq = T("ssq", [128, NT], F32)
    rstd = T("rstd", [128, NT], F32)
    zf = T("zf", [128, 8, NT], F32)
    Gm = T("Gm", [128, 8, NT], F32)
    fA = T("fA", [128, 8, NT], F32)
    fB = T("fB", [128, 8, NT], F32)
    ftot = T("ftot", [128, 8, NT], F32)
    bfg = T("bfg", [128, 8], F32)
    gtn = [T("gtn%d" % i, [128, 128], BF16) for i in range(2)]
    c8 = arenaA[0:8, 0:S]
    pscol = T("pscol", [128, 4], F32)
    wpool_b = T("wpool_b", [128, 4, 128], BF16)
    gb = arenaA[:, 16384:18432].bitcast(F32)
    bada = [arenaA[:, 8192 + i * 1024:8192 + (i + 1) * 1024].bitcast(F32) for i in range(2)]
    wada = [arenaA[:, i * 4096:(i + 1) * 4096].rearrange("p (k n) -> p k n", k=8) for i in range(2)]
    LG = T("LG", [128, NT, 36], F32)
    brb = T("brb", [128, 36], F32)
    wr_b = T("wr_b", [128, 8, 36], BF16)
    pos_i = [T("pos_i%d" % k, [128, NT], I32) for k in range(2)]
    comb = [T("comb%d" % k, [128, NT], F32) for k in range(2)]
    idxW = T("idxW", [128, NTILE], I32)
    rrow = T("rrow", [128, 512], F32)
    rbs = T("rbs", [128, 512], F32)

    def load_const(dst, src, cast, key):
        q = "pool" if cast else "sp"
        pg.dma(q, lambda e: e.dma_start(out=dst, in_=src), writes=[key])

    load_const(ident_f[:], din["k_ident"], False, "ident_f")
    load_const(triu_f[:], din["k_triu"], False, "triu_f")
    load_const(ones_f[:], din["k_ones"], False, "ones_f")
    load_const(ident_b[:], din["k_ident"], True, "ident_b")
    load_const(ltri_b[:], din["k_ltri"], True, "ltri_b")
    load_const(ones_b[:], din["k_ones"], True, "ones_b")
    load_const(negm_b[:], din["k_negmask"], True, "negm_b")
    load_const(band_b[:], din["k_band"].rearrange("g s t -> s g t"), True, "band_b")
    load_const(invc_f[:], din["k_invc"].rearrange("g s t -> s g t"), False, "invc_f")
    load_const(jv_f[:], din["k_jv"], False, "jv_f")
    load_const(pidx_f[:], din["k_pidx"], False, "pidx_f")
    pg.op("pool", lambda e: e.memset(zero_b[:], 0.0), writes=["zero_b"])
    pg.op("pool", lambda e: e.memset(epsc[:], EPS), writes=["epsc"])
    pg.op("pool", lambda e: e.memset(c8, 8.0), writes=["c8"])
    pg.dma("sp", lambda e: e.dma_start(out=kT_d[:, 64, :], in_=c8), reads=["c8"], writes=["kaug"])
    for i in range(2):
        odd = i % 2
        pg.op("pool", lambda e, i=i: e.memset(vaug[i][:], 0.0), writes=[("vaug", i)])
        col = 0 if odd else 64
        pg.op("pool", lambda e, i=i, col=col: e.memset(vaug[i][:, :, col:col + 64], 1.0), writes=[("vaug", i)])
    pg.dma("sp", lambda e: e.dma_start(out=ccol[:], in_=din["c_col"]), writes=["ccol"])
    pg.op("act", lambda e: e.activation(cact[:], ccol[:], AF.Silu), reads=["ccol"], writes=["cact"])
    for k in range(8):
        pg.op("act", lambda e, k=k: e.activation(cb[:, k, :], zero_b[:], AF.Identity, bias=cact[:, k:k + 1], scale=1.0),
              reads=["zero_b", "cact"], writes=["cb"])

    conv_state = {"n": 0}
    conv_items = []
    if stop is None or stop == "p4":
        for l in range(nl):
            for e_ in range(32):
                for piece in range(3):
                    conv_items.append((l, e_, piece))

    def conv_step(nsteps=1):
        for _ in range(nsteps):
            if not conv_items:
                return
            l, e_, piece = conv_items.pop(0)
            n = conv_state["n"]
            conv_state["n"] += 1
            sl = n % 2
            src = din["wexp"][l, e_ * 128:(e_ + 1) * 128, piece * 2048:(piece + 1) * 2048]
            dst = wbf_d[l][e_ * 128:(e_ + 1) * 128, piece * 2048:(piece + 1) * 2048]
            pg.dma("pool", lambda e, sl=sl, src=src: e.dma_start(out=stage[sl][:], in_=src),
                   writes=[("stage", sl)], bg=True)
            pg.dma("sp", lambda e, sl=sl, dst=dst: e.dma_start(out=dst, in_=stage[sl][:]),
                   reads=[("stage", sl)], writes=[("wbf", l, e_, piece)], bg=True)

    def rms_sq(xs, xkey, i):
        pg.op("act", lambda e: e.activation(sq[:], xs, AF.Square, accum_out=ssq[:, i:i + 1]),
              reads=[xkey], writes=["sq", ("ssq", i)])
        pg.op("act", lambda e: e.activation(rstd[:, i:i + 1], ssq[:, i:i + 1], AF.Ln, bias=epsc[:, 0:1], scale=1.0 / D),
              reads=[("ssq", i), "epsc"], writes=[("rstd", i)])
        pg.op("act", lambda e: e.activation(rstd[:, i:i + 1], rstd[:, i:i + 1], AF.Exp, scale=-0.5), reads=[("rstd", i)], writes=[("rstd", i)])

    def rms_apply(xs, xkey, i, Gap, SHap, hdst, hkey, tslot):
        tm = tmpf[tslot]
        pg.op("dve", lambda e: e.scalar_tensor_tensor(tm[:], xs, rstd[:, i:i + 1], Gap, ALU.mult, ALU.mult),
              reads=[xkey, ("rstd", i), "mod"], writes=[("tmpf", tslot)])
        pg.op("pool", lambda e: e.tensor_tensor(hdst, tm[:], SHap, ALU.add),
              reads=[("tmpf", tslot), "mod"], writes=[hkey])

    catT = arenaA[:].rearrange("p (c t) -> p c t", c=8)

    NMC = 24
    mod_d = scratch("mod_d", [1, 6 * D], F32)
    mwc = [xt[i][:].bitcast(BF16).rearrange("p (k n) -> p k n", k=8) for i in range(3)]
    mbc = [xnt[0][0:1, i * 256:(i + 1) * 256] for i in range(2)]
    msg = [xnt[1][0:1, i * 256:(i + 1) * 256] for i in range(2)]

    def m_load(l1, c):
        pg.dma("pool", lambda e: e.dma_start(
            out=mwc[c % 3], in_=din["w_ada"][l1].rearrange("(k p) n -> p k n", p=128)[:, :, c * 256:(c + 1) * 256]),
            writes=[("mwc", c % 3)])
        pg.dma("sp", lambda e: e.dma_start(out=mbc[c % 2], in_=din["b_ada"][l1:l1 + 1, c * 256:(c + 1) * 256]),
               writes=[("mbc", c % 2)])

    def m_comp(c, bank):
        pb = psb[bank]
        for k in range(8):
            pg.op("pe", lambda e: e.matmul(pb[:, 0:256], cb[:, k, :], mwc[c % 3][:, k, :], start=(k == 0), stop=(k == 7)),
                  reads=["cb", ("mwc", c % 3)], writes=[("ps", bank)])
        pg.op("dve", lambda e: e.tensor_tensor(msg[c % 2], pb[0:1, 0:256], mbc[c % 2], ALU.add),
              reads=[("ps", bank), ("mbc", c % 2)], writes=[("msg", c % 2)])
        pg.dma("sp", lambda e: e.dma_start(out=mod_d[0:1, c * 256:(c + 1) * 256], in_=msg[c % 2]),
               reads=[("msg", c % 2)], writes=[("mod_d", c)])

    for l in range(nl):
        xsrc = din["x"] if l == 0 else xres
        pg.fence(fsc[:, 0:1])
        for n in range(12 if l == 0 else 0):
            sl = n % 2
            pg.dma("pool", lambda e, sl=sl, n=n: e.dma_start(
                out=wada[sl], in_=din["w_ada"][l].rearrange("(k p) n -> p k n", p=128)[:, :, n * 512:(n + 1) * 512]),
                writes=[("wada", sl)])
            pg.dma("sp", lambda e, sl=sl, n=n: e.dma_start(
                out=bada[sl], in_=din["b_ada"][l:l + 1, n * 512:(n + 1) * 512].partition_broadcast(128)[:, 0, :]),
                writes=[("bada", sl)])
            pb, pk = ps_next()
            for k in range(8):
                pg.op("pe", lambda e, pb=pb, sl=sl, k=k: e.matmul(pb[:], cb[:, k, :], wada[sl][:, k, :], start=(k == 0), stop=(k == 7)),
                      reads=["cb", ("wada", sl)], writes=[pk])
            pg.op("dve", lambda e, pb=pb, sl=sl, n=n: e.tensor_tensor(mod[:, n * 512:(n + 1) * 512], pb[:], bada[sl], ALU.add),
                  reads=[pk, ("bada", sl)], writes=["mod"])
        def mod_gains(l_):
            for (goff, gname) in ((1024, "norm_mix_g"), (4096, "norm_ffn_g")):
                pg.dma("sp", lambda e: e.dma_start(out=gb, in_=din[gname][l_:l_ + 1, :].partition_broadcast(128)[:, 0, :]),
                       writes=["gb"])
                pg.op("dve", lambda e: e.scalar_tensor_tensor(mod[:, goff:goff + D], mod[:, goff:goff + D], 1.0, gb, ALU.add, ALU.mult),
                      reads=["gb", "mod"], writes=["mod"])

        def mod_reload(l_):
            for q in range(4):
                pg.dma("sp" if q % 2 == 0 else "pool", lambda e: e.dma_start(
                    out=mod[:, q * 1536:(q + 1) * 1536], in_=mod_d[0:1, q * 1536:(q + 1) * 1536].partition_broadcast(128)[:, 0, :]),
                    reads=[("mod_d", c) for c in range(NMC)] + ["mod"], writes=["mod"])
            mod_gains(l_)
        if l == 0:
            mod_gains(l)
        SH1, G1, GTM = mod[:, 0:D], mod[:, D:2 * D], mod[:, 2 * D:3 * D]
        SH2, G2, GTF = mod[:, 3 * D:4 * D], mod[:, 4 * D:5 * D], mod[:, 5 * D:6 * D]
        if "dbg_mod" in dbg_d and l == 0:
            pg.dma("sp", lambda e: e.dma_start(out=dbg_d["dbg_mod"], in_=mod[:]), reads=["mod"], writes=["dbgmod"])

        w_in_b = arenaB[:].rearrange("p (k n) -> p k n", k=8)
        wsrc = din["w_in"][l].rearrange("(k p) n -> p k n", p=128)
        pg.dma("pool", lambda e: e.dma_start(out=w_in_b[:, :, 0:1024], in_=wsrc[:, :, 0:1024]), writes=["w_in_b"])
        pg.dma("pool", lambda e: e.dma_start(out=w_in_b[:, :, 1024:2056], in_=wsrc[:, :, 1024:2056]), writes=["w_in_b"])
        pg.dma("pool", lambda e: e.dma_start(out=wpool_b[:], in_=din["w_pool"][l].rearrange("g c d -> c g d")), writes=["wpool_b"])
        pg.dma("sp", lambda e: e.dma_start(out=pscol[:], in_=din["ps_col"][l]), writes=["pscol"])
        pg.dma("sp", lambda e: e.dma_start(out=bfg[:], in_=din["b_fgate"][l:l + 1, :].partition_broadcast(128)[:, 0, :]), writes=["bfg"])
        hT = [arenaA[:, i * 4096:(i + 1) * 4096].rearrange("p (k t) -> p k t", k=8) for i in range(2)]
        uring = arenaA[:, 8192:12288].rearrange("p (r n) -> p r n", r=8)
        vtile = [arenaA[:, 12288 + i * 512:12288 + (i + 1) * 512] for i in range(2)]
        qks = [arenaA[:, 13312 + i * 512:13312 + (i + 1) * 512] for i in range(2)]
        dft = [arenaA[:, 14336 + i * 512:14336 + (i + 1) * 512] for i in range(2)]
        def p1_L(i):
            xs_ = i % 3
            pg.dma("sp", lambda e: e.dma_start(out=xt[xs_][:], in_=xsrc[i * 128:(i + 1) * 128, :]),
                   reads=[("xres", i)], writes=[("xt", xs_)])

        def p1_A0(i):
            rms_sq(xt[i % 3][:], ("xt", i % 3), i)

        def p1_A1(i):
            rms_apply(xt[i % 3][:], ("xt", i % 3), i, G1, SH1, hb[i % 2][:], ("hb", i % 2), i % 2)

        def p1_A2(i):
            B, r = divmod(i, 4)
            pb, pk = ps_next()
            pTv = pb[:].bitcast(BF16).rearrange("p (k t) -> p k t", k=8)
            for k in range(8):
                pg.op("pe", lambda e: e.transpose(pTv[:, k, :], hb[i % 2][:, k * 128:(k + 1) * 128], ident_b[:]),
                      reads=[("hb", i % 2), "ident_b"], writes=[pk])
            pg.op("act", lambda e: e.activation(hT[B % 2][:, :, r * 128:(r + 1) * 128], pTv, AF.Copy),
                  reads=[pk], writes=[("hT", B % 2, r)])

        def p1_B(i):
            B, r = divmod(i, 4)
            pv, pvk = ps_next()
            pu, puk = ps_next()
            pf, pfk = ps_next()
            for (pp, ppk, c0, c1) in ((pv, pvk, 1024, 1536), (pu, puk, 1544, 2056), (pf, pfk, 1536, 1544)):
                for k in range(8):
                    pg.op("pe", lambda e: e.matmul(
                        pp[:, 0:c1 - c0], hT[B % 2][:, k, r * 128:(r + 1) * 128], w_in_b[:, k, c0:c1], start=(k == 0), stop=(k == 7)),
                        reads=[("hT", B % 2, r), "w_in_b"], writes=[ppk])
            vs = i % 2
            pg.op("dve", lambda e: e.tensor_copy(vtile[vs], pv[:]), reads=[pvk], writes=[("vtile", vs)])
            pg.dma("sp", lambda e: e.dma_start(out=v_d[i * 128:(i + 1) * 128, :], in_=vtile[vs]),
                   reads=[("vtile", vs)], writes=[("v_d", i)])
            us = i % 8
            pg.op("act", lambda e: e.activation(uring[:, us, :], pu[:], AF.Copy), reads=[puk], writes=[("uring", us)])
            pg.op("dve", lambda e: e.tensor_copy(zf[:, :, i], pf[:, 0:8]), reads=[pfk], writes=[("zf", i)])

        def p1_QK(B):
            for c in range(8):
                pq, pqk = ps_next()
                for k in range(8):
                    pg.op("pe", lambda e: e.matmul(pq[:], w_in_b[:, k, c * 128:(c + 1) * 128], hT[B % 2][:, k, :],
                                                   start=(k == 0), stop=(k == 7)),
                          reads=[("hT", B % 2, r_) for r_ in range(4)] + ["w_in_b"], writes=[pqk])
                qs = c % 2
                if c % 2 == 0:
                    pg.op("dve", lambda e: e.tensor_copy(qks[qs], pq[:]), reads=[pqk], writes=[("qks", qs)])
                else:
                    pg.op("act", lambda e: e.activation(qks[qs], pq[:], AF.Copy), reads=[pqk], writes=[("qks", qs)])
                dst = qT_d if c < 4 else kT_d
                h0 = (c % 4) * 2
                for hh in range(2):
                    pg.dma("sp", lambda e: e.dma_start(
                        out=dst[h0 + hh, 0:64, B * 512:(B + 1) * 512], in_=qks[qs][hh * 64:(hh + 1) * 64, :]),
                        reads=[("qks", qs)], writes=[("qkd", c, hh, B)])

        def p1_POOL(B):
            for g in range(4):
                w = POOL_W[g]
                pd, pdk = ps_next()
                for r in range(4):
                    i = B * 4 + r
                    bsel = g * 3 + (2 if i == 0 else 0)
                    pg.op("pe", lambda e: e.matmul(
                        pd[:, r * 128:(r + 1) * 128], uring[:, i % 8, g * 128:(g + 1) * 128], band_b[:, bsel, :], start=True, stop=(i == 0)),
                        reads=[("uring", i % 8), "band_b"], writes=[pdk])
                    if i > 0:
                        pg.op("pe", lambda e: e.matmul(
                            pd[:, r * 128:(r + 1) * 128], uring[:, (i - 1) % 8, g * 128:(g + 1) * 128], band_b[:, g * 3 + 1, :], start=False, stop=True),
                            reads=[("uring", (i - 1) % 8), "band_b"], writes=[pdk])
                ds = g % 2
                if B == 0:
                    pg.op("dve", lambda e: e.tensor_tensor(dft[ds][:, 0:128], pd[:, 0:128], invc_f[:, g, :], ALU.mult),
                          reads=[pdk, "invc_f"], writes=[("dft", ds)])
                    pg.op("act", lambda e: e.activation(dft[ds][:, 128:512], pd[:, 128:512], AF.Copy, scale=1.0 / w),
                          reads=[pdk], writes=[("dft", ds)])
                else:
                    pg.op("act", lambda e: e.activation(dft[ds], pd[:], AF.Copy, scale=1.0 / w),
                          reads=[pdk], writes=[("dft", ds)])
                pm, pmk = ps_next()
                pg.op("pe", lambda e: e.matmul(pm[:], wpool_b[:, g, :], dft[ds], start=True, stop=True),
                      reads=["wpool_b", ("dft", ds)], writes=[pmk])
                pg.op("act", lambda e: e.activation(catT[:, 4 + g, B * 512:(B + 1) * 512], pm[:], AF.Identity, scale=pscol[:, g:g + 1]),
                      reads=[pmk, "pscol"], writes=[("catT", 4 + g, B * 4 + r_) for r_ in range(4)])

        def p1_FG():
            zkeys = [("zf", i) for i in range(NT)]
            pg.op("dve", lambda e: e.tensor_tensor(zf[:], zf[:], bfg[:].unsqueeze(2).to_broadcast([128, 8, NT]), ALU.add),
                  reads=zkeys + ["bfg"], writes=["zfa"])
            pg.op("act", lambda e: e.activation(fA[:], zf[:], AF.Exp, scale=-1.0), reads=["zfa"], writes=["fA"])
            pg.op("act", lambda e: e.activation(fB[:], fA[:], AF.Ln, bias=1.0), reads=["fA"], writes=["fB"])
            pl, plk = ps_next()
            pt2, ptk = ps_next()
            nlf2 = fB[:].rearrange("p h t -> p (h t)")
            pg.op("pe", lambda e: e.matmul(pl[:, 0:256], triu_f[:], nlf2, start=True, stop=True), reads=["fB", "triu_f"], writes=[plk])
            pg.op("pe", lambda e: e.matmul(pt2[:, 0:256], ones_f[:], nlf2, start=True, stop=True), reads=["fB", "ones_f"], writes=[ptk])
            pg.op("dve", lambda e: e.tensor_copy(ftot[:].rearrange("p h t -> p (h t)"), pt2[:, 0:256]), reads=[ptk], writes=["ftot"])
            pg.op("dve", lambda e: e.tensor_copy(fA[:], ftot[:]), reads=["ftot", "fA"], writes=["fA"])
            cur, oth, ck, ok_ = fA, fB, "fA", "fB"
            for sft in (1, 2, 4, 8, 16):
                pg.op("dve", lambda e, cur=cur, oth=oth, sft=sft: e.tensor_copy(oth[:, :, 0:sft], cur[:, :, 0:sft]),
                      reads=[ck, plk, ptk], writes=[ok_])
                pg.op("dve", lambda e, cur=cur, oth=oth, sft=sft: e.tensor_tensor(oth[:, :, sft:NT], cur[:, :, sft:NT], cur[:, :, 0:NT - sft], ALU.add),
                      reads=[ck], writes=[ok_])
                cur, oth, ck, ok_ = oth, cur, ok_, ck
            pg.op("dve", lambda e, cur=cur: e.tensor_tensor(Gm[:], cur[:], ftot[:], ALU.subtract), reads=[ck, "ftot"], writes=["Gm"])
            pg.op("dve", lambda e: e.tensor_tensor(Gm[:].rearrange("p h t -> p (h t)"), Gm[:].rearrange("p h t -> p (h t)"), pl[:, 0:256], ALU.add),
                  reads=["Gm", plk], writes=["Gm"])
            if "dbg_G" in dbg_d and l == 0:
                pg.dma("sp", lambda e: e.dma_start(out=dbg_d["dbg_G"], in_=Gm[:]), reads=["Gm"], writes=["dbgG"])
            for half in range(2):
                pt3, pt3k = ps_next()
                pg.op("pe", lambda e, pt3=pt3, half=half: e.transpose(pt3[:, 0:128], Gm[:, half * 4:(half + 1) * 4, :].rearrange("p h t -> p (h t)"), ident_f[:]),
                      reads=["Gm", "ident_f"], writes=[pt3k])
                pg.op("act", lambda e, pt3=pt3, half=half: e.activation(gtn[half][:], pt3[:, 0:128], AF.Copy, scale=-1.0),
                      reads=[pt3k], writes=[("gtn", half)])
                for hl in range(4):
                    h = half * 4 + hl
                    pg.dma("sp", lambda e, h=h, hl=hl, half=half: e.dma_start(
                        out=qT_d[h, 64, :].rearrange("(i t) -> i t", t=128), in_=gtn[half][hl * 32:(hl + 1) * 32, :]),
                        reads=[("gtn", half)], writes=[("qaug", h)])

        for it in range(-3, NT + 1):
            if 0 <= it + 3 < NT:
                p1_L(it + 3)
            if 0 <= it + 2 < NT:
                p1_A0(it + 2)
            if 0 <= it + 1 < NT:
                p1_A1(it + 1)
            if 0 <= it < NT:
                p1_A2(it)
            if 0 <= it - 1 < NT:
                p1_B(it - 1)
                if it - 1 == NT - 1:
                    p1_FG()
                if (it - 1) % 4 == 3:
                    p1_QK((it - 1) // 4)
                    p1_POOL((it - 1) // 4)
        if stop == "p1":
            if "dbg_cat" in dbg_d:
                pg.dma("sp", lambda e: e.dma_start(out=dbg_d["dbg_cat"], in_=catT), reads=[("catT", c, i) for c in range(8) for i in range(NT)], writes=["dbgcat"])
            break

        pg.fence(fsc[:, 1:2])
        qkh = arenaB[:].rearrange("p (s n) -> p s n", s=4)
        PT = [hb[0][:, 0:512], hb[0][:, 512:1024], hb[1][:, 0:512], hb[1][:, 512:1024]]
        po = [psb[0], psb[1], psb[2]]
        NSR = 4
        s_ring = [psb[3 + i] for i in range(NSR)]
        do_m = (l + 1 < nl)
        LA = 2
        all_qk_keys = [("qkd", c, hh, B) for c in range(8) for hh in range(2) for B in range(8)] + [("qaug", h) for h in range(8)] + ["kaug"]

        def load_head(h):
            sl = h % 2
            vs = h % 2
            pg.dma("sp", lambda e: e.dma_start(out=qkh[0:65, sl * 2 + 0, 0:S], in_=qT_d[h]), reads=all_qk_keys, writes=[("qh", sl)])
            pg.dma("sp", lambda e: e.dma_start(out=qkh[0:65, sl * 2 + 1, 0:S], in_=kT_d[h]), reads=all_qk_keys, writes=[("kh", sl)])
            off = 64 if h % 2 else 0
            pg.dma("sp", lambda e: e.dma_start(out=vaug[vs][:, :, off:off + 64], in_=v_d.rearrange("(i p) c -> p i c", p=128)[:, :, h * 64:(h + 1) * 64]),
                   reads=[("v_d", i) for i in range(NT)], writes=[("vaug", vs)])

        tiles = []
        blk = 0
        for h in range(NH):
            for I in range(8):
                n_kt = 4 * (I + 1)
                for j in range(n_kt - 1, -1, -1):
                    tiles.append((h, I, j, blk, j == n_kt - 1, j == 0))
                blk += 1

        def emit_S(n):
            h, I, j, blk_, first, last = tiles[n]
            sl = h % 2
            qh = qkh[0:65, sl * 2 + 0, 0:S]
            kh = qkh[0:65, sl * 2 + 1, 0:S]
            r = j - 4 * I
            q0 = max(r, 0) * 128
            sb_ = s_ring[n % NSR]
            sk = ("ps", 3 + n % NSR)
            kt = kh[:, j * 128:(j + 1) * 128]
            rd = [("qh", sl), ("kh", sl)]
            if r >= 0:
                pg.op("pe", lambda e: e.matmul(sb_[:, q0:q0 + 128], kt, qh[:, I * 512 + q0:I * 512 + q0 + 128], start=True, stop=False),
                      reads=rd, writes=[sk])
                pg.op("pe", lambda e: e.matmul(sb_[:, q0:q0 + 128], negm_b[:], ident_b[:], start=False, stop=True),
                      reads=["negm_b", "ident_b"], writes=[sk])
                if q0 + 128 < 512:
                    pg.op("pe", lambda e: e.matmul(sb_[:, q0 + 128:512], kt, qh[:, I * 512 + q0 + 128:(I + 1) * 512], start=True, stop=True),
                          reads=rd, writes=[sk])
            else:
                pg.op("pe", lambda e: e.matmul(sb_[:], kt, qh[:, I * 512:(I + 1) * 512], start=True, stop=True), reads=rd, writes=[sk])

        def emit_exp_pv(n):
            h, I, j, blk_, first, last = tiles[n]
            sl = h % 2
            vs = h % 2
            qh = qkh[0:65, sl * 2 + 0, 0:S]
            r = j - 4 * I
            q0 = max(r, 0) * 128
            sb_ = s_ring[n % NSR]
            sk = ("ps", 3 + n % NSR)
            ptl = PT[n % 4]
            ptk_ = ("PT", n % 4)
            pob = po[blk_ % 3]
            pok = ("ps", blk_ % 3)
            if first:
                pg.op("pe", lambda e: e.matmul(pob[:], zero_b[0:65, :], qh[:, I * 512:(I + 1) * 512], start=True, stop=False),
                      reads=["zero_b", ("qh", sl)], writes=[pok])
            pg.op("act", lambda e: e.activation(ptl[:, q0:512], sb_[:, q0:512], AF.Exp, bias=Gm[:, h, j:j + 1], scale=0.125),
                  reads=[sk, "Gm"], writes=[ptk_])
            pg.op("pe", lambda e: e.matmul(pob[:, q0:512], vaug[vs][:, j, :], ptl[:, q0:512], start=False, stop=last),
                  reads=[("vaug", vs), ptk_], writes=[pok])

        def emit_norm(h, I, blk_):
            odd = h % 2
            rows = slice(64, 128) if odd else slice(0, 64)
            drows = slice(0, 64) if odd else slice(64, 128)
            pob = po[blk_ % 3]
            rr = tmpf[(blk_ % 3) // 2][:, ((blk_ % 3) % 2) * 512:((blk_ % 3) % 2 + 1) * 512]
            pg.op("dve", lambda e: e.reciprocal(rr[drows, :], pob[drows, :]), reads=[("ps", blk_ % 3)], writes=[("rrow", blk_ % 3)])
            pg.op("dve", lambda e: e.tensor_tensor(catT[rows, h // 2, I * 512:(I + 1) * 512], pob[rows, :], rr[drows, :], ALU.mult),
                  reads=[("ps", blk_ % 3), ("rrow", blk_ % 3)], writes=[("catT", h // 2, I * 4 + r_, odd) for r_ in range(4)])

        load_head(0)
        load_head(1)
        ntl = len(tiles)
        for n in range(min(LA, ntl)):
            emit_S(n)
        for n in range(ntl):
            h, I, j, blk_, first, last = tiles[n]
            if n + LA < ntl:
                emit_S(n + LA)
            if first and (blk_ % 2 == 0):
                conv_step(3)
            if first and (blk_ % 2 == 1) and do_m:
                ev = blk_ // 2
                if ev == 0:
                    m_load(l + 1, 0)
                    m_load(l + 1, 1)
                if ev < NMC:
                    m_comp(ev, 7)
                    if ev + 2 < NMC:
                        m_load(l + 1, ev + 2)
            emit_exp_pv(n)
            if last:
                emit_norm(h, I, blk_)
                if I == 7 and h + 2 < NH:
                    load_head(h + 2)
        if stop == "p2":
            if "dbg_cat" in dbg_d:
                pg.dma("sp", lambda e: e.dma_start(out=dbg_d["dbg_cat"], in_=catT),
                       reads=[("catT", c, i) for c in range(4, 8) for i in range(NT)] + [("catT", c, i, o_) for c in range(4) for i in range(NT) for o_ in range(2)],
                       writes=["dbgcat"])
            break

        pg.fence(fsc[:, 2:3])
        w_out_b = arenaB[:, 0:8 * D].rearrange("p (k n) -> p k n", k=8)
        pg.dma("pool", lambda e: e.dma_start(out=w_out_b, in_=din["w_out"][l].rearrange("(k p) n -> p k n", p=128)), writes=["w_out_b"])
        pg.dma("pool", lambda e: e.dma_start(out=wr_b[:], in_=din["w_r"][l].rearrange("(k p) n -> p k n", p=128)), writes=["wr_b"])
        pg.dma("sp", lambda e: e.dma_start(out=brb[:], in_=din["b_r"][l:l + 1, :].partition_broadcast(128)[:, 0, :]), writes=["brb"])
        hT2 = [arenaB[:, 8192 + i * 1024:8192 + (i + 1) * 1024].rearrange("p (k t) -> p k t", k=8) for i in range(2)]
        p3_pm = {}

        def p3_M(i):
            xs_ = i % 3
            pg.dma("sp", lambda e: e.dma_start(out=xt[xs_][:], in_=xsrc[i * 128:(i + 1) * 128, :]), reads=[("xres", i)], writes=[("xt", xs_)])
            cat_keys = [("catT", c, i) for c in range(4, 8)] + [("catT", c, i, o_) for c in range(4) for o_ in range(2)]
            pmx = []
            for half in range(2):
                bnk = (i % 3) * 2 + half
                pm_, pmk_ = psb[bnk], ("ps", bnk)
                pmx.append((pm_, pmk_))
                for c in range(8):
                    pg.op("pe", lambda e: e.matmul(
                        pm_[:], catT[:, c, i * 128:(i + 1) * 128], w_out_b[:, c, half * 512:(half + 1) * 512], start=(c == 0), stop=(c == 7)),
                        reads=cat_keys + ["w_out_b"], writes=[pmk_])
            p3_pm[i] = pmx

        def p3_R1(i):
            xs_ = i % 3
            xn_ = i % 2
            pmx = p3_pm.pop(i)
            for half in range(2):
                pm_, pmk_ = pmx[half]
                pg.op("dve", lambda e: e.tensor_tensor(
                    xnt[xn_][:, half * 512:(half + 1) * 512], pm_[:], GTM[:, half * 512:(half + 1) * 512], ALU.mult),
                    reads=[pmk_, "mod"], writes=[("xnt", xn_)])
            pg.op("dve", lambda e: e.tensor_tensor(xnt[xn_][:], xnt[xn_][:], xt[xs_][:], ALU.add),
                  reads=[("xnt", xn_), ("xt", xs_)], writes=[("xnt", xn_)])
            pg.dma("sp", lambda e: e.dma_start(out=xres[i * 128:(i + 1) * 128, :], in_=xnt[xn_][:]),
                   reads=[("xnt", xn_)], writes=[("xres", i)])
            rms_sq(xnt[xn_][:], ("xnt", xn_), i)

        def p3_R2(i):
            xn_ = i % 2
            rms_apply(xnt[xn_][:], ("xnt", xn_), i, G2, SH2, hb[i % 2][:], ("hb", i % 2), i % 2)
            pg.dma("sp", lambda e: e.dma_start(out=h_d[i * 128:(i + 1) * 128, :], in_=hb[i % 2][:]),
                   reads=[("hb", i % 2)], writes=[("h_d", i)])

        def p3_R3(i):
            pb, pk = psb[6], ("ps", 6)
            pTv = pb[:].bitcast(BF16).rearrange("p (k t) -> p k t", k=8)
            for k in range(8):
                pg.op("pe", lambda e: e.transpose(pTv[:, k, :], hb[i % 2][:, k * 128:(k + 1) * 128], ident_b[:]),
                      reads=[("hb", i % 2), "ident_b"], writes=[pk])
            pg.op("act", lambda e: e.activation(hT2[i % 2], pTv, AF.Copy), reads=[pk], writes=[("hT2", i % 2)])

        def p3_LG(i):
            plg, plgk = psb[7], ("ps", 7)
            for k in range(8):
                pg.op("pe", lambda e: e.matmul(plg[:, 0:36], hT2[i % 2][:, k, :], wr_b[:, k, :], start=(k == 0), stop=(k == 7)),
                      reads=[("hT2", i % 2), "wr_b"], writes=[plgk])
            pg.op("dve", lambda e: e.tensor_tensor(LG[:, i, :], plg[:, 0:36], brb[:], ALU.add),
                  reads=[plgk, "brb"], writes=[("LG", i)])

        for i in range(min(3, NT)):
            p3_M(i)
        for it in range(NT + 3):
            if it < NT:
                p3_R1(it)
            if it + 3 < NT:
                p3_M(it + 3)
            if 0 <= it - 1 < NT:
                p3_R2(it - 1)
            if 0 <= it - 2 < NT:
                p3_R3(it - 2)
            if 0 <= it - 3 < NT:
                p3_LG(it - 3)
        if "dbg_LG" in dbg_d and l == 0:
            pg.dma("sp", lambda e: e.dma_start(out=dbg_d["dbg_LG"], in_=LG[:]), reads=[("LG", i) for i in range(NT)], writes=["dbgLG"])

        pg.fence(fsc[:, 3:4])
        RA = arenaA[:].bitcast(F32)

        def rt(idx_, n=1024):
            return RA[:, idx_ * 1024:idx_ * 1024 + n]
        vm = rt(0).rearrange("p (i g) -> p i g", i=NT)
        vm4 = rt(0).rearrange("p (i g e) -> p i g e", i=NT, g=4)
        oh1 = rt(1).rearrange("p (i g) -> p i g", i=NT)
        oh2 = rt(2).rearrange("p (i g) -> p i g", i=NT)
        vm2 = rt(3).rearrange("p (i g) -> p i g", i=NT)
        tA = rt(4).rearrange("p (i g) -> p i g", i=NT)
        tB = rt(5).rearrange("p (i g) -> p i g", i=NT)
        rin = rt(6).rearrange("p (i g) -> p i g", i=NT)
        tot = rt(7).rearrange("p (i g) -> p i g", i=NT)
        cmp3 = RA[:, 8 * 1024:8 * 1024 + NTILE * 32].rearrange("p (j e) -> p j e", e=32)
        sm = RA[:, 11 * 1024:12 * 1024]
        ohb = arenaA[:, 12 * 2048:12 * 2048 + 1024]
        gmax, sumg, topp = sm[:, 0:32], sm[:, 32:64], sm[:, 64:96]
        m1, m2, e2 = sm[:, 96:128], sm[:, 128:160], sm[:, 160:192]
        ew1, pos1f, pos2f = sm[:, 192:224], sm[:, 224:256], sm[:, 256:288]
        cntv, pcv, t1v = sm[:, 288:320], sm[:, 320:352], sm[:, 352:384]
        offA, offB, endo = sm[:, 384:416], sm[:, 416:448], sm[:, 448:480]
        ohg = sm[:, 480:608].rearrange("p (i g) -> p i g", g=4)
        egx = sm[:, 608:736].rearrange("p (i g) -> p i g", g=4)
        pen = sm[:, 736:864].rearrange("p (i g) -> p i g", g=4)
        tef = sm[:, 864:960]
        LGg = LG[:, :, 0:4]
        LE4 = LG[:, :, 4:36].rearrange("p i (g e) -> p i g e", g=4)
        RK = "route"
        lgk = [("LG", i) for i in range(NT)]
        NSL = 8
        hsl = [arenaB[:, i * 1024:(i + 1) * 1024] for i in range(NSL)]

        def hsl_load(i):
            pg.dma("sp", lambda e: e.dma_start(out=hsl[i % NSL], in_=h_d[i * 128:(i + 1) * 128, :]),
                   reads=[("h_d", i)], writes=[("hsl", i % NSL)])
        for i in range(NSL):
            hsl_load(i)

        def dv(fn, reads=(), eng="dve"):
            pg.op(eng, fn, reads=[RK] + list(reads), writes=[RK])
        dv(lambda e: e.tensor_reduce(gmax, LGg, AX.X, ALU.max), reads=lgk)
        dv(lambda e: e.tensor_tensor(ohg, LGg, gmax.unsqueeze(2).to_broadcast([128, NT, 4]), ALU.is_equal))
        dv(lambda e: e.tensor_tensor(egx, LGg, gmax.unsqueeze(2).to_broadcast([128, NT, 4]), ALU.subtract))
        dv(lambda e: e.activation(egx, egx, AF.Exp), eng="act")
        dv(lambda e: e.tensor_reduce(sumg, egx, AX.X, ALU.add))
        dv(lambda e: e.reciprocal(topp, sumg))
        dv(lambda e: e.tensor_scalar(pen, ohg, BIG, -BIG, ALU.mult, ALU.add))
        dv(lambda e: e.tensor_tensor(vm4, LE4, pen.unsqueeze(3).to_broadcast([128, NT, 4, 8]), ALU.add))
        dv(lambda e: e.tensor_reduce(m1, vm, AX.X, ALU.max))
        dv(lambda e: e.tensor_tensor(oh1, vm, m1.unsqueeze(2).to_broadcast([128, NT, 32]), ALU.is_equal))
        dv(lambda e: e.scalar_tensor_tensor(vm2, oh1, -BIG, vm, ALU.mult, ALU.add))
        dv(lambda e: e.tensor_reduce(m2, vm2, AX.X, ALU.max))
        dv(lambda e: e.tensor_tensor(oh2, vm2, m2.unsqueeze(2).to_broadcast([128, NT, 32]), ALU.is_equal))
        dv(lambda e: e.tensor_tensor(e2, m2, m1, ALU.subtract))
        dv(lambda e: e.activation(e2, e2, AF.Exp), eng="act")
        dv(lambda e: e.tensor_scalar(ew1, e2, 1.0, None, ALU.add))
        dv(lambda e: e.reciprocal(ew1, ew1))
        dv(lambda e: e.tensor_tensor(comb[0][:], topp, ew1, ALU.mult))
        dv(lambda e: e.tensor_tensor(comb[1][:], comb[0][:], e2, ALU.mult))
        dv(lambda e: e.tensor_tensor(ohb.rearrange("p (i g) -> p i g", i=NT), oh1, oh2, ALU.add))
        for (dst, lhs, lk) in ((rin, ltri_b, "ltri_b"), (tot, ones_b, "ones_b")):
            for half in range(2):
                pb, pk = ps_next()
                pg.op("pe", lambda e, pb=pb, lhs=lhs, half=half: e.matmul(pb[:], lhs[:], ohb[:, half * 512:(half + 1) * 512], start=True, stop=True),
                      reads=[RK, lk], writes=[pk])
                pg.op("dve", lambda e, pb=pb, dst=dst, half=half: e.tensor_copy(dst[:, half * 16:(half + 1) * 16, :].rearrange("p i g -> p (i g)"), pb[:]),
                      reads=[pk, RK], writes=[RK])
        dv(lambda e: e.tensor_copy(tA, tot))
        cur, oth = tA, tB
        for sft in (1, 2, 4, 8, 16):
            dv(lambda e, cur=cur, oth=oth, sft=sft: e.tensor_copy(oth[:, 0:sft, :], cur[:, 0:sft, :]))
            dv(lambda e, cur=cur, oth=oth, sft=sft: e.tensor_tensor(oth[:, sft:NT, :], cur[:, sft:NT, :], cur[:, 0:NT - sft, :], ALU.add))
            cur, oth = oth, cur
        inc3 = cur
        dv(lambda e: e.tensor_copy(cntv, inc3[:, NT - 1, :]))
        dv(lambda e: e.tensor_scalar(t1v, cntv, 127.0, None, ALU.add))
        dv(lambda e: e.tensor_scalar(pcv, t1v, 1.0 / 128, -0.49609375, ALU.mult, ALU.add))
        dv(lambda e: e.tensor_scalar(pcv, pcv, 8388608.0, None, ALU.add))
        dv(lambda e: e.tensor_scalar(pcv, pcv, -8388608.0, 128.0, ALU.add, ALU.mult))
        dv(lambda e: e.tensor_copy(offA, pcv))
        c2, o2 = offA, offB
        for sft in (1, 2, 4, 8, 16):
            dv(lambda e, c2=c2, o2=o2, sft=sft: e.tensor_copy(o2[:, 0:sft], c2[:, 0:sft]))
            dv(lambda e, c2=c2, o2=o2, sft=sft: e.tensor_tensor(o2[:, sft:32], c2[:, sft:32], c2[:, 0:32 - sft], ALU.add))
            c2, o2 = o2, c2
        dv(lambda e, c2=c2: e.tensor_copy(endo, c2))
        dv(lambda e: e.tensor_tensor(offA if c2 is not offA else offB, endo, pcv, ALU.subtract))
        offx = offA if c2 is not offA else offB
        dv(lambda e: e.tensor_tensor(oth, inc3, tot, ALU.subtract))
        dv(lambda e: e.tensor_tensor(oth, oth, rin, ALU.add))
        dv(lambda e: e.tensor_tensor(oth, oth, offx.unsqueeze(1).to_broadcast([128, NT, 32]), ALU.add))
        dv(lambda e: e.tensor_tensor(vm, oth, oh1, ALU.mult))
        dv(lambda e: e.tensor_reduce(pos1f, vm, AX.X, ALU.add))
        dv(lambda e: e.tensor_tensor(vm, oth, oh2, ALU.mult))
        dv(lambda e: e.tensor_reduce(pos2f, vm, AX.X, ALU.add))
        dv(lambda e: e.tensor_copy(pos_i[0][:], pos1f))
        dv(lambda e: e.tensor_copy(pos_i[1][:], pos2f))
        dv(lambda e: e.tensor_tensor(cmp3, endo.unsqueeze(1).to_broadcast([128, NTILE, 32]), jv_f[:].unsqueeze(2).to_broadcast([128, NTILE, 32]), ALU.is_le),
           reads=["jv_f"])
        dv(lambda e: e.tensor_reduce(tef, cmp3, AX.X, ALU.add))
        dv(lambda e: e.tensor_scalar(tef, tef, 128.0, None, ALU.mult))
        dv(lambda e: e.tensor_scalar(tef, tef, pidx_f[:, 0:1], None, ALU.add), reads=["pidx_f"])
        dv(lambda e: e.tensor_copy(idxW[:], tef))
        if "dbg_route" in dbg_d and l == 0:
            dr = dbg_d["dbg_route"]
            for n_, src_ in enumerate((pos1f, pos2f, comb[0][:], comb[1][:], m1, m2)):
                pg.dma("sp", lambda e, n_=n_, src_=src_: e.dma_start(out=dr[:, n_, :], in_=src_), reads=[RK], writes=[("dbgr", n_)])
            if "dbg_te" in dbg_d:
                pg.dma("sp", lambda e: e.dma_start(out=dbg_d["dbg_te"], in_=tef), reads=[RK], writes=["dbgte"])
        for i in range(NT):
            for k in range(2):
                pg.dma("pool", lambda e: e.indirect_dma_start(
                    out=hs_d, out_offset=bass.IndirectOffsetOnAxis(ap=pos_i[k][:, i:i + 1], axis=0), in_=hsl[i % NSL], in_offset=None),
                    reads=[("hsl", i % NSL), RK], writes=[("hs_d", i, k)])
            if i + NSL < NT:
                hsl_load(i + NSL)
        if stop == "p3":
            break

        pg.fence(fsc[:, 4:5])
        NW = 5
        Wt = [arenaA[:, i * 6144:(i + 1) * 6144] for i in range(NW)]
        o4 = 0
        hst = [arenaB[:, o4 + i * 1024:o4 + (i + 1) * 1024] for i in range(3)]
        o4 += 3 * 1024
        hsT = [arenaB[:, o4 + i * 1024:o4 + (i + 1) * 1024].rearrange("p (k t) -> p k t", k=8) for i in range(3)]
        o4 += 3 * 1024
        sa = [arenaB[:, o4 + i * 512:o4 + (i + 1) * 512].bitcast(F32) for i in range(3)]
        o4 += 3 * 512
        actb = [arenaB[:, o4 + i * 256:o4 + (i + 1) * 256] for i in range(3)]
        o4 += 3 * 256
        actT = [arenaB[:, o4 + i * 256:o4 + (i + 1) * 256].rearrange("p (k t) -> p k t", k=2) for i in range(3)]
        o4 += 3 * 256
        yt = [xnt[0], xnt[1]]
        hs_keys = [("hs_d", i, k) for i in range(NT) for k in range(2)]
        wbf_keys = [("wbf", l, e_, p_) for e_ in range(32) for p_ in range(3)]
        while conv_items and conv_items[0][0] <= l:
            conv_step(1)

        def moe_load(j):
            pg.dma("pool", lambda e: e.indirect_dma_start(
                out=Wt[j % NW], out_offset=None, in_=wbf_d[l], in_offset=bass.IndirectOffsetOnAxis(ap=idxW[:, j:j + 1], axis=0),
                bounds_check=pg.wbound, oob_is_err=False),
                reads=wbf_keys + [RK], writes=[("Wt", j % NW)])
            pg.dma("sp", lambda e: e.dma_start(out=hst[j % 3], in_=hs_d[j * 128:(j + 1) * 128, :]),
                   reads=hs_keys, writes=[("hst", j % 3)])

        def moe_S1(j):
            pb, pk = ps_next()
            pTv = pb[:].bitcast(BF16).rearrange("p (k t) -> p k t", k=8)
            for k in range(8):
                pg.op("pe", lambda e: e.transpose(pTv[:, k, :], hst[j % 3][:, k * 128:(k + 1) * 128], ident_b[:]),
                      reads=[("hst", j % 3), "ident_b"], writes=[pk])
            pg.op("act", lambda e: e.activation(hsT[j % 3], pTv, AF.Copy), reads=[pk], writes=[("hsT", j % 3)])

        def moe_S2(j):
            w_ = Wt[j % NW]
            pgu, pguk = ps_next()
            for k in range(8):
                pg.op("pe", lambda e: e.matmul(pgu[:], hsT[j % 3][:, k, :], w_[:, k * 512:(k + 1) * 512], start=(k == 0), stop=(k == 7)),
                      reads=[("hsT", j % 3), ("Wt", j % NW)], writes=[pguk])
            pg.op("act", lambda e: e.activation(sa[j % 3], pgu[:, 0:256], AF.Silu), reads=[pguk], writes=[("sa", j % 3)])
            pg.op("dve", lambda e: e.tensor_tensor(actb[j % 3], sa[j % 3], pgu[:, 256:512], ALU.mult),
                  reads=[pguk, ("sa", j % 3)], writes=[("actb", j % 3)])

        def moe_S3(j):
            pb2, pk2 = ps_next()
            pT2 = pb2[:].bitcast(BF16)[:, 0:256].rearrange("p (k t) -> p k t", k=2)
            for k in range(2):
                pg.op("pe", lambda e: e.transpose(pT2[:, k, :], actb[j % 3][:, k * 128:(k + 1) * 128], ident_b[:]),
                      reads=[("actb", j % 3), "ident_b"], writes=[pk2])
            pg.op("dve", lambda e: e.tensor_copy(actT[j % 3], pT2), reads=[pk2], writes=[("actT", j % 3)])

        def moe_S4(j):
            w_ = Wt[j % NW]
            for half in range(2):
                py, pyk = ps_next()
                for k in range(2):
                    pg.op("pe", lambda e: e.matmul(
                        py[:], actT[j % 3][:, k, :], w_[:, 4096 + k * 1024 + half * 512:4096 + k * 1024 + (half + 1) * 512], start=(k == 0), stop=(k == 1)),
                        reads=[("actT", j % 3), ("Wt", j % NW)], writes=[pyk])
                pg.op("dve", lambda e: e.tensor_tensor(yt[j % 2][:, half * 512:(half + 1) * 512], py[:], GTF[:, half * 512:(half + 1) * 512], ALU.mult),
                      reads=[pyk, "mod"], writes=[("yt", j % 2, half)])
            pg.dma("sp", lambda e: e.dma_start(out=ys_d[j * 128:(j + 1) * 128, :], in_=yt[j % 2][:]),
                   reads=[("yt", j % 2, 0), ("yt", j % 2, 1)], writes=[("ys_d", j)])

        PF = 2
        for j in range(min(PF, NTILE)):
            moe_load(j)
        for s_ in range(NTILE + 3):
            if 0 <= s_ - 3 < NTILE:
                moe_S4(s_ - 3)
            if s_ + PF < NTILE:
                moe_load(s_ + PF)
            if s_ < NTILE:
                moe_S1(s_)
            if 0 <= s_ - 1 < NTILE:
                moe_S2(s_ - 1)
            if 0 <= s_ - 2 < NTILE:
                moe_S3(s_ - 2)
        if stop == "p4":
            break

        pg.fence(fsc[:, 5:6])
        ys_keys = [("ys_d", j) for j in range(NTILE)]
        y12 = [[arenaA[:, (2 * s_ + k) * 2048:(2 * s_ + k + 1) * 2048].bitcast(F32) for k in range(2)] for s_ in range(3)]
        last = (l == nl - 1)
        if last:
            pg.dma("sp", lambda e: e.dma_start(out=gb, in_=din["norm_final_g"][0:1, :].partition_broadcast(128)[:, 0, :]), writes=["gb"])
        else:
            mod_reload(l + 1)

        def comb_load(i):
            for k in range(2):
                pg.dma("pool", lambda e, i=i, k=k: e.indirect_dma_start(
                    out=y12[i % 3][k], out_offset=None, in_=ys_d, in_offset=bass.IndirectOffsetOnAxis(ap=pos_i[k][:, i:i + 1], axis=0)),
                    reads=ys_keys + [RK], writes=[("y12", i % 3, k)])
            pg.dma("sp", lambda e, i=i: e.dma_start(out=xt[i % 3][:], in_=xres[i * 128:(i + 1) * 128, :]),
                   reads=[("xres", i)], writes=[("xt", i % 3)])

        comb_load(0)
        comb_load(1)
        for i in range(NT):
            if i + 2 < NT:
                comb_load(i + 2)
            ya, yb_ = y12[i % 3]
            ts = i % 2
            pg.op("act", lambda e, ya=ya, i=i, ts=ts: e.activation(tmpf[ts][:], ya, AF.Copy, scale=comb[0][:, i:i + 1]),
                  reads=[("y12", i % 3, 0), RK], writes=[("tmpf", ts)])
            pg.op("dve", lambda e, yb_=yb_, i=i, ts=ts: e.scalar_tensor_tensor(tmpf[ts][:], yb_, comb[1][:, i:i + 1], tmpf[ts][:], ALU.mult, ALU.add),
                  reads=[("y12", i % 3, 1), RK, ("tmpf", ts)], writes=[("tmpf", ts)])
            xn_ = i % 2
            pg.op("dve", lambda e, ts=ts, i=i, xn_=xn_: e.tensor_tensor(xnt[xn_][:], tmpf[ts][:], xt[i % 3][:], ALU.add),
                  reads=[("tmpf", ts), ("xt", i % 3)], writes=[("xnt", xn_)])
            if not last:
                pg.dma("sp", lambda e, xn_=xn_, i=i: e.dma_start(out=xres[i * 128:(i + 1) * 128, :], in_=xnt[xn_][:]),
                       reads=[("xnt", xn_)], writes=[("xres", i)])
            else:
                pg.op("act", lambda e, xn_=xn_, i=i: e.activation(sq[:], xnt[xn_][:], AF.Square, accum_out=ssq[:, i:i + 1]),
                      reads=[("xnt", xn_)], writes=["sq", ("ssq", i)])
                pg.op("dve", lambda e, i=i: e.tensor_scalar(rstd[:, i:i + 1], ssq[:, i:i + 1], 1.0 / D, EPS, ALU.mult, ALU.add),
                      reads=[("ssq", i)], writes=[("rstd", i)])
                pg.op("act", lambda e, i=i: e.activation(rstd[:, i:i + 1], rstd[:, i:i + 1], AF.Ln), reads=[("rstd", i)], writes=[("rstd", i)])
                pg.op("act", lambda e, i=i: e.activation(rstd[:, i:i + 1], rstd[:, i:i + 1], AF.Exp, scale=-0.5), reads=[("rstd", i)], writes=[("rstd", i)])
                pg.op("dve", lambda e, xn_=xn_, i=i: e.scalar_tensor_tensor(xnt[xn_][:], xnt[xn_][:], rstd[:, i:i + 1], gb, ALU.mult, ALU.mult),
                      reads=[("xnt", xn_), ("rstd", i), "gb"], writes=[("xnt", xn_)])
                pg.dma("sp", lambda e, xn_=xn_, i=i: e.dma_start(out=out_d[i * 128:(i + 1) * 128, :], in_=xnt[xn_][:]),
                       reads=[("xnt", xn_)], writes=[("out", i)])
    pg.emit()
    st.close()
    return nc, pg


def prep_inputs(inp):
    f = lambda a: np.ascontiguousarray(np.asarray(a, dtype=np.float32))
    shared = {}
    for k in ("norm_mix_g", "norm_ffn_g", "w_ada", "b_ada", "w_in", "b_fgate", "w_pool", "w_out"):
        shared[k] = f(inp[k])
    shared["norm_final_g"] = f(inp["norm_final_g"]).reshape(1, D)
    shared["ps_col"] = f(np.asarray(inp["pool_scale"]).reshape(NL, 4, 128).transpose(0, 2, 1))
    wre = np.asarray(inp["w_router_expert"]).transpose(0, 2, 1, 3).reshape(NL, D, 32)
    shared["w_r"] = f(np.concatenate([np.asarray(inp["w_router_group"]), wre], axis=2))
    shared["b_r"] = f(np.concatenate([np.asarray(inp["b_router_group"]), np.asarray(inp["b_router_expert"]).reshape(NL, 32)], axis=1))
    wg = np.asarray(inp["w_expert_gate"]).reshape(NL, 32, 8, 128, 256).transpose(0, 1, 3, 2, 4)
    wu = np.asarray(inp["w_expert_up"]).reshape(NL, 32, 8, 128, 256).transpose(0, 1, 3, 2, 4)
    wgu = np.concatenate([wg, wu], axis=4).reshape(NL, 32, 128, 4096)
    wd = np.asarray(inp["w_expert_down"]).reshape(NL, 32, 2, 128, 1024).transpose(0, 1, 3, 2, 4).reshape(NL, 32, 128, 2048)
    shared["wexp"] = f(np.concatenate([wgu, wd], axis=3).reshape(NL, 4096, 6144))
    for k, v in make_consts().items():
        shared["k_" + k] = f(v)
    x = np.asarray(inp["x"], dtype=np.float32)
    c = np.asarray(inp["c"], dtype=np.float32)
    per_core = []
    for b in range(8):
        m = dict(shared)
        m["x"] = np.ascontiguousarray(x[b])
        m["c_col"] = np.ascontiguousarray(c[b].reshape(8, 128).T)
        per_core.append(m)
    return per_core


_CACHE = {}


def kernel(**inputs):
    if "nc" not in _CACHE:
        _CACHE["nc"] = build()[0]
    nc = _CACHE["nc"]
    in_maps = prep_inputs(inputs)
    res = run_bass_kernel_spmd(nc, in_maps, core_ids=list(range(8)))
    return np.stack([np.asarray(r["out"], dtype=np.float32) for r in res.results], axis=0)
```

```python
import contextlib
import numpy as np
import concourse.bass as bass
import concourse.mybir as mybir
from concourse.bass_utils import run_bass_kernel_spmd

F32 = mybir.dt.float32
BF16 = mybir.dt.bfloat16
I32 = mybir.dt.int32
ALU = mybir.AluOpType
AF = mybir.ActivationFunctionType
AX = mybir.AxisListType

S = 4096
D = 1024
NT = 32
NL = 2
NH = 8
NTILE = 96
EPS = 1e-6
BIG = 30000.0
POOL_W = (2, 4, 8, 16)

COMPUTE = ("pe", "act", "dve", "pool")
DMAQ = ("sp", "pool", "act")


class _Rec:
    def __init__(self):
        self.call = None

    def __getattr__(self, name):
        def f(*a, **kw):
            assert self.call is None
            self.call = (name, a, kw)
            return self
        return f


class _Late:
    def __init__(self):
        self.v = None


def _replay(fn):
    rec = _Rec()
    fn(rec)
    name, a, kw = rec.call
    return lambda e: getattr(e, name)(*a, **{k: (v.v if isinstance(v, _Late) else v) for k, v in kw.items()})


class _Op:
    __slots__ = ("eng", "fn", "deps", "is_dma", "sig", "has_dep", "bg")

    def __init__(self, eng, fn, is_dma, bg):
        self.eng = eng
        self.fn = _replay(fn)
        self.deps = set()
        self.is_dma = is_dma
        self.sig = None
        self.has_dep = False
        self.bg = bg


class Prog:
    def __init__(self, nc, dma_ring=8):
        self.nc = nc
        self.ops = []
        self.last_writer = {}
        self.readers = {}
        self.dma_ring = dma_ring
        self.fence_op = None
        self.since_fence_dma = []
        self.last_eng_op = {}
        self.wbound = _Late()

    def _add(self, eng, fn, reads, writes, is_dma, bg):
        o = _Op(eng, fn, is_dma, bg)
        for k in reads:
            w = self.last_writer.get(k)
            if w is not None:
                o.deps.add(w)
        for k in writes:
            w = self.last_writer.get(k)
            if w is not None:
                o.deps.add(w)
            for r in self.readers.get(k, ()):
                o.deps.add(r)
        for k in writes:
            self.last_writer[k] = o
            self.readers[k] = []
        for k in reads:
            self.readers.setdefault(k, []).append(o)
        if not bg:
            if self.fence_op is not None:
                o.deps.add(self.fence_op)
            if is_dma:
                self.since_fence_dma.append(o)
            else:
                self.last_eng_op[eng] = o
        o.deps.discard(o)
        self.ops.append(o)
        return o

    def op(self, eng, fn, reads=(), writes=(), bg=False):
        return self._add(eng, fn, reads, writes, False, bg)

    def dma(self, eng, fn, reads=(), writes=(), bg=False):
        return self._add(eng, fn, reads, writes, True, bg)

    def fence(self, scratch):
        o = _Op("pool", lambda e: e.memset(scratch, 0.0), False, False)
        if self.fence_op is not None:
            o.deps.add(self.fence_op)
        for d in self.since_fence_dma:
            o.deps.add(d)
        for d in self.last_eng_op.values():
            o.deps.add(d)
        self.since_fence_dma = []
        self.last_eng_op = {"pool": o}
        self.fence_op = o
        self.ops.append(o)
        return o

    def emit(self):
        nc = self.nc
        ops = self.ops
        for o in ops:
            for d in o.deps:
                if d.is_dma:
                    d.has_dep = True
                elif d.eng == o.eng and d.eng == "pe" and not o.is_dma:
                    pass
                else:
                    d.has_dep = True
        stack = contextlib.ExitStack()
        sems = {e: stack.enter_context(nc.semaphore("s_" + e)) for e in COMPUTE}
        dsem = {q: [stack.enter_context(nc.semaphore("d_%s%d" % (q, i))) for i in range(self.dma_ring)]
                for q in DMAQ}
        cnt = {e: 0 for e in COMPUTE}
        dcnt = {q: [0] * self.dma_ring for q in DMAQ}
        dnum = {q: 0 for q in DMAQ}
        engs = ("pe", "act", "dve", "pool", "sp")
        waited = {e: {} for e in engs}
        streams = {e: [] for e in engs}
        for o in ops:
            eng = o.eng
            need = {}
            for d in o.deps:
                if d.sig is None:
                    continue
                if (not d.is_dma) and d.eng == eng and eng == "pe" and not o.is_dma:
                    continue
                s, v = d.sig
                if need.get(s, (None, 0))[1] < v:
                    need[s] = (s, v)
            if o.is_dma:
                r = dnum[eng] % self.dma_ring
                dnum[eng] += 1
                s = dsem[eng][r]
                prev = dcnt[eng][r]
                if prev > 0 and need.get(s, (None, 0))[1] < prev:
                    need[s] = (s, prev)
                dcnt[eng][r] += 16
                o.sig = (s, dcnt[eng][r])
                inc = (s, 16)
            elif o.has_dep:
                cnt[eng] += 1
                o.sig = (sems[eng], cnt[eng])
                inc = (sems[eng], 1)
            else:
                inc = None
            w = waited[eng]
            wl = []
            for s, v in need.values():
                if w.get(s, 0) >= v:
                    continue
                w[s] = v
                wl.append((s, v))
            streams[eng].append((wl, o.fn, inc))
        fin = []
        for q in DMAQ:
            for i in range(self.dma_ring):
                if dcnt[q][i] > 0:
                    fin.append((dsem[q][i], dcnt[q][i]))
        for e in COMPUTE:
            if cnt[e] > 0:
                fin.append((sems[e], cnt[e]))
        self.n_instr = {e: len(v) for e, v in streams.items()}

        def run(engname, e):
            if engname == "pool":
                r = e.alloc_register("wbound")
                e.reg_mov(r, 4095)
                self.wbound.v = r
            for wl, fn, inc in streams[engname]:
                for s, v in wl:
                    e.wait_ge(s, v)
                ins = fn(e)
                if inc is not None:
                    ins.then_inc(inc[0], inc[1])
            if engname == "sp":
                for s, v in fin:
                    e.wait_ge(s, v)

        with nc.Block() as block:
            @block.tensor
            def _(e):
                run("pe", e)

            @block.scalar
            def _(e):
                run("act", e)

            @block.vector
            def _(e):
                run("dve", e)

            @block.gpsimd
            def _(e):
                run("pool", e)

            @block.sync
            def _(e):
                run("sp", e)
        stack.close()


def make_consts():
    c = {}
    idx = np.arange(128)
    s_ = idx[:, None]
    t_ = idx[None, :]
    c["ident"] = np.eye(128, dtype=np.float32)
    c["triu"] = (s_ <= t_).astype(np.float32)
    c["ltri"] = (s_ < t_).astype(np.float32)
    c["ones"] = np.ones((128, 128), np.float32)
    c["negmask"] = np.where(t_ > s_, -BIG, 0.0).astype(np.float32)
    band = np.zeros((12, 128, 128), np.float32)
    invc = np.zeros((4, 128, 128), np.float32)
    for g, w in enumerate(POOL_W):
        same = ((s_ <= t_) & (s_ > t_ - w)).astype(np.float32)
        same[idx, idx] -= w
        prev = (s_ - 128 > t_ - w).astype(np.float32)
        first = ((s_ <= t_) & (s_ > t_ - w)).astype(np.float32)
        cntv = np.minimum(idx + 1, w).astype(np.float32)
        first[idx, idx] -= cntv
        band[g * 3 + 0] = same
        band[g * 3 + 1] = prev
        band[g * 3 + 2] = first
        invc[g] = np.broadcast_to((1.0 / cntv)[None, :], (128, 128))
    c["band"] = band
    c["invc"] = invc
    c["jv"] = np.broadcast_to((np.arange(NTILE, dtype=np.float32) * 128.0)[None, :], (128, NTILE)).copy()
    c["pidx"] = np.arange(128, dtype=np.float32)[:, None].copy()
    return c


CONST_SHAPES = {"ident": [128, 128], "triu": [128, 128], "ltri": [128, 128], "ones": [128, 128],
                "negmask": [128, 128], "band": [12, 128, 128], "invc": [4, 128, 128],
                "jv": [128, NTILE], "pidx": [128, 1]}

IN_SHAPES = {
    "x": [S, D], "c_col": [128, 8], "norm_mix_g": [NL, D], "norm_ffn_g": [NL, D], "norm_final_g": [1, D],
    "w_ada": [NL, D, 6 * D], "b_ada": [NL, 6 * D], "w_in": [NL, D, 2056], "b_fgate": [NL, 8],
    "w_pool": [NL, 4, 128, 128], "ps_col": [NL, 128, 4], "w_out": [NL, D, D],
    "w_r": [NL, D, 36], "b_r": [NL, 36], "wexp": [NL, 4096, 6144],
}


def build(nl=NL, stop=None, dbg=()):
    nc = bass.Bass("TRN2", target_bir_lowering=False)
    st = contextlib.ExitStack()
    din = {}
    need_in = set(IN_SHAPES)
    if stop in ("p1", "p2"):
        need_in -= {"w_out", "w_r", "b_r", "wexp", "norm_final_g"}
    if stop == "p3":
        need_in -= {"wexp", "norm_final_g"}
    for k in sorted(need_in):
        din[k] = nc.dram_tensor(k, IN_SHAPES[k], F32, kind="ExternalInput").ap()
    for k, shp in CONST_SHAPES.items():
        din["k_" + k] = nc.dram_tensor("k_" + k, shp, F32, kind="ExternalInput").ap()
    out_d = nc.dram_tensor("out", [S, D], F32, kind="ExternalOutput").ap()

    def scratch(name, shape, dt):
        kind = "ExternalOutput" if name in dbg else "Internal"
        return nc.dram_tensor(name, shape, dt, kind=kind).ap()

    xres = scratch("xres", [S, D], F32)
    qT_d = scratch("qT_d", [NH, 65, S], BF16)
    kT_d = scratch("kT_d", [NH, 65, S], BF16)
    v_d = scratch("v_d", [S, 512], BF16)
    h_d = scratch("h_d", [S, D], BF16)
    hs_d = scratch("hs_d", [NTILE * 128, D], BF16)
    ys_d = scratch("ys_d", [NTILE * 128, D], F32)
    wbf_d = [scratch("wbf%d" % l, [4096, 6144], BF16) for l in range(nl)]
    dbg_d = {}
    for name, shp, dt in (("dbg_cat", [128, 8, S], BF16), ("dbg_G", [128, 8, NT], F32),
                          ("dbg_mod", [128, 6 * D], F32), ("dbg_LG", [128, NT, 36], F32),
                          ("dbg_route", [128, 6, NT], F32), ("dbg_te", [128, NTILE], F32)):
        if name in dbg:
            dbg_d[name] = nc.dram_tensor(name, shp, dt, kind="ExternalOutput").ap()

    def T(name, shape, dt):
        return st.enter_context(nc.sbuf_tensor(name, shape, dt))

    pg = Prog(nc)
    psb = [st.enter_context(nc.psum_tensor("psb%d" % i, [128, 512], F32)) for i in range(8)]
    ps_rr = [0]

    def ps_next():
        b = ps_rr[0] % 8
        ps_rr[0] += 1
        return psb[b], ("ps", b)

    arenaA = T("arenaA", [128, 8 * S], BF16)
    arenaB = T("arenaB", [128, 8 * 2056], BF16)
    mod = T("mod", [128, 6 * D], F32)
    vaug = [T("vaug%d" % i, [128, NT, 128], BF16) for i in range(2)]
    stage = [T("stage%d" % i, [128, 2048], BF16) for i in range(2)]
    xt = [T("xt%d" % i, [128, D], F32) for i in range(3)]
    tmpf = [T("tmpf%d" % i, [128, D], F32) for i in range(2)]
    xnt = [T("xnt%d" % i, [128, D], F32) for i in range(2)]
    hb = [T("hb%d" % i, [128, D], BF16) for i in range(2)]
    sq = T("sq", [128, D], BF16)
    fsc = T("fsc", [128, 8], F32)
    ident_f = T("ident_f", [128, 128], F32)
    triu_f = T("triu_f", [128, 128], F32)
    ones_f = T("ones_f", [128, 128], F32)
    ident_b = T("ident_b", [128, 128], BF16)
    ltri_b = T("ltri_b", [128, 128], BF16)
    ones_b = T("ones_b", [128, 128], BF16)
    negm_b = T("negm_b", [128, 128], BF16)
    band_b = T("band_b", [128, 12, 128], BF16)
    invc_f = T("invc_f", [128, 4, 128], F32)
    jv_f = T("jv_f", [128, NTILE], F32)
    pidx_f = T("pidx_f", [128, 1], F32)
    zero_b = T("zero_b", [128, 128], BF16)
    epsc = T("epsc", [128, 1], F32)
    cb = T("cb", [128, 8, 128], BF16)
    ccol = T("ccol", [128, 8], F32)
    cact = T("cact", [128, 8], F32)
    ssq = T("ssq", [128, NT], F32)
    rstd = T("rstd", [128, NT], F32)
    zf = T("zf", [128, 8, NT], F32)
    Gm = T("Gm", [128, 8, NT], F32)
    fA = T("fA", [128, 8, NT], F32)
    fB = T("fB", [128, 8, NT], F32)
    ftot = T("ftot", [128, 8, NT], F32)
    bfg = T("bfg", [128, 8], F32)
    gtn = [T("gtn%d" % i, [128, 128], BF16) for i in range(2)]
    c8 = arenaA[0:8, 0:S]
    pscol = T("pscol", [128, 4], F32)
    wpool_b = T("wpool_b", [128, 4, 128], BF16)
    gb = arenaA[:, 16384:18432].bitcast(F32)
    bada = [arenaA[:, 8192 + i * 1024:8192 + (i + 1) * 1024].bitcast(F32) for i in range(2)]
    wada = [arenaA[:, i * 4096:(i + 1) * 4096].rearrange("p (k n) -> p k n", k=8) for i in range(2)]
    LG = T("LG", [128, NT, 36], F32)
    brb = T("brb", [128, 36], F32)
    wr_b = T("wr_b", [128, 8, 36], BF16)
    pos_i = [T("pos_i%d" % k, [128, NT], I32) for k in range(2)]
    comb = [T("comb%d" % k, [128, NT], F32) for k in range(2)]
    idxW = T("idxW", [128, NTILE], I32)
    rrow = T("rrow", [128, 512], F32)
    rbs = T("rbs", [128, 512], F32)

    def load_const(dst, src, cast, key):
        q = "pool" if cast else "sp"
        pg.dma(q, lambda e: e.dma_start(out=dst, in_=src), writes=[key])

    load_const(ident_f[:], din["k_ident"], False, "ident_f")
    load_const(triu_f[:], din["k_triu"], False, "triu_f")
    load_const(ones_f[:], din["k_ones"], False, "ones_f")
    load_const(ident_b[:], din["k_ident"], True, "ident_b")
    load_const(ltri_b[:], din["k_ltri"], True, "ltri_b")
    load_const(ones_b[:], din["k_ones"], True, "ones_b")
    load_const(negm_b[:], din["k_negmask"], True, "negm_b")
    load_const(band_b[:], din["k_band"].rearrange("g s t -> s g t"), True, "band_b")
    load_const(invc_f[:], din["k_invc"].rearrange("g s t -> s g t"), False, "invc_f")
    load_const(jv_f[:], din["k_jv"], False, "jv_f")
    load_const(pidx_f[:], din["k_pidx"], False, "pidx_f")
    pg.op("pool", lambda e: e.memset(zero_b[:], 0.0), writes=["zero_b"])
    pg.op("pool", lambda e: e.memset(epsc[:], EPS), writes=["epsc"])
    pg.op("pool", lambda e: e.memset(c8, 8.0), writes=["c8"])
    pg.dma("sp", lambda e: e.dma_start(out=kT_d[:, 64, :], in_=c8), reads=["c8"], writes=["kaug"])
    for i in range(2):
        odd = i % 2
        pg.op("pool", lambda e, i=i: e.memset(vaug[i][:], 0.0), writes=[("vaug", i)])
        col = 0 if odd else 64
        pg.op("pool", lambda e, i=i, col=col: e.memset(vaug[i][:, :, col:col + 64], 1.0), writes=[("vaug", i)])
    pg.dma("sp", lambda e: e.dma_start(out=ccol[:], in_=din["c_col"]), writes=["ccol"])
    pg.op("act", lambda e: e.activation(cact[:], ccol[:], AF.Silu), reads=["ccol"], writes=["cact"])
    for k in range(8):
        pg.op("act", lambda e, k=k: e.activation(cb[:, k, :], zero_b[:], AF.Identity, bias=cact[:, k:k + 1], scale=1.0),
              reads=["zero_b", "cact"], writes=["cb"])

    conv_state = {"n": 0}
    conv_items = []
    if stop is None or stop == "p4":
        for l in range(nl):
            for e_ in range(32):
                for piece in range(3):
                    conv_items.append((l, e_, piece))

    def conv_step(nsteps=1):
        for _ in range(nsteps):
            if not conv_items:
                return
            l, e_, piece = conv_items.pop(0)
            n = conv_state["n"]
            conv_state["n"] += 1
            sl = n % 2
            src = din["wexp"][l, e_ * 128:(e_ + 1) * 128, piece * 2048:(piece + 1) * 2048]
            dst = wbf_d[l][e_ * 128:(e_ + 1) * 128, piece * 2048:(piece + 1) * 2048]
            pg.dma("pool", lambda e, sl=sl, src=src: e.dma_start(out=stage[sl][:], in_=src),
                   writes=[("stage", sl)], bg=True)
            pg.dma("sp", lambda e, sl=sl, dst=dst: e.dma_start(out=dst, in_=stage[sl][:]),
                   reads=[("stage", sl)], writes=[("wbf", l, e_, piece)], bg=True)

    def rms_sq(xs, xkey, i):
        pg.op("act", lambda e: e.activation(sq[:], xs, AF.Square, accum_out=ssq[:, i:i + 1]),
              reads=[xkey], writes=["sq", ("ssq", i)])
        pg.op("act", lambda e: e.activation(rstd[:, i:i + 1], ssq[:, i:i + 1], AF.Ln, bias=epsc[:, 0:1], scale=1.0 / D),
              reads=[("ssq", i), "epsc"], writes=[("rstd", i)])
        pg.op("act", lambda e: e.activation(rstd[:, i:i + 1], rstd[:, i:i + 1], AF.Exp, scale=-0.5), reads=[("rstd", i)], writes=[("rstd", i)])

    def rms_apply(xs, xkey, i, Gap, SHap, hdst, hkey, tslot):
        tm = tmpf[tslot]
        pg.op("dve", lambda e: e.scalar_tensor_tensor(tm[:], xs, rstd[:, i:i + 1], Gap, ALU.mult, ALU.mult),
              reads=[xkey, ("rstd", i), "mod"], writes=[("tmpf", tslot)])
        pg.op("pool", lambda e: e.tensor_tensor(hdst, tm[:], SHap, ALU.add),
              reads=[("tmpf", tslot), "mod"], writes=[hkey])

    catT = arenaA[:].rearrange("p (c t) -> p c t", c=8)

    NMC = 24
    mod_d = scratch("mod_d", [1, 6 * D], F32)
    mwc = [xt[i][:].bitcast(BF16).rearrange("p (k n) -> p k n", k=8) for i in range(3)]
    mbc = [xnt[0][0:1, i * 256:(i + 1) * 256] for i in range(2)]
    msg = [xnt[1][0:1, i * 256:(i + 1) * 256] for i in range(2)]

    def m_load(l1, c):
        pg.dma("pool", lambda e: e.dma_start(
            out=mwc[c % 3], in_=din["w_ada"][l1].rearrange("(k p) n -> p k n", p=128)[:, :, c * 256:(c + 1) * 256]),
            writes=[("mwc", c % 3)])
        pg.dma("sp", lambda e: e.dma_start(out=mbc[c % 2], in_=din["b_ada"][l1:l1 + 1, c * 256:(c + 1) * 256]),
               writes=[("mbc", c % 2)])

    def m_comp(c, bank):
        pb = psb[bank]
        for k in range(8):
            pg.op("pe", lambda e: e.matmul(pb[:, 0:256], cb[:, k, :], mwc[c % 3][:, k, :], start=(k == 0), stop=(k == 7)),
                  reads=["cb", ("mwc", c % 3)], writes=[("ps", bank)])
        pg.op("dve", lambda e: e.tensor_tensor(msg[c % 2], pb[0:1, 0:256], mbc[c % 2], ALU.add),
              reads=[("ps", bank), ("mbc", c % 2)], writes=[("msg", c % 2)])
        pg.dma("sp", lambda e: e.dma_start(out=mod_d[0:1, c * 256:(c + 1) * 256], in_=msg[c % 2]),
               reads=[("msg", c % 2)], writes=[("mod_d", c)])

    for l in range(nl):
        xsrc = din["x"] if l == 0 else xres
        pg.fence(fsc[:, 0:1])
        for n in range(4 if l == 0 else 0):
            sl = n % 2
            pg.dma("pool", lambda e, sl=sl, n=n: e.dma_start(
                out=wada[sl], in_=din["w_ada"][l].rearrange("(k p) n -> p k n", p=128)[:, :, n * 512:(n + 1) * 512]),
                writes=[("wada", sl)])
            pg.dma("sp", lambda e, sl=sl, n=n: e.dma_start(
                out=bada[sl], in_=din["b_ada"][l:l + 1, n * 512:(n + 1) * 512].partition_broadcast(128)[:, 0, :]),
                writes=[("bada", sl)])
            pb, pk = ps_next()
            for k in range(8):
                pg.op("pe", lambda e, pb=pb, sl=sl, k=k: e.matmul(pb[:], cb[:, k, :], wada[sl][:, k, :], start=(k == 0), stop=(k == 7)),
                      reads=["cb", ("wada", sl)], writes=[pk])
            pg.op("dve", lambda e, pb=pb, sl=sl, n=n: e.tensor_tensor(mod[:, n * 512:(n + 1) * 512], pb[:], bada[sl], ALU.add),
                  reads=[pk, ("bada", sl)], writes=["mod"])
        def mod_gains(l_):
            for (goff, gname) in ((1024, "norm_mix_g"), (4096, "norm_ffn_g")):
                pg.dma("sp", lambda e: e.dma_start(out=gb, in_=din[gname][l_:l_ + 1, :].partition_broadcast(128)[:, 0, :]),
                       writes=["gb"])
                pg.op("dve", lambda e: e.scalar_tensor_tensor(mod[:, goff:goff + D], mod[:, goff:goff + D], 1.0, gb, ALU.add, ALU.mult),
                      reads=["gb", "mod"], writes=["mod"])

        def mod_reload(l_):
            for q in range(4):
                pg.dma("sp" if q % 2 == 0 else "pool", lambda e: e.dma_start(
                    out=mod[:, q * 1536:(q + 1) * 1536], in_=mod_d[0:1, q * 1536:(q + 1) * 1536].partition_broadcast(128)[:, 0, :]),
                    reads=[("mod_d", c) for c in range(NMC)] + ["mod"], writes=["mod"])
            mod_gains(l_)
        if l == 0:
            pg.dma("sp", lambda e: e.dma_start(out=gb, in_=din["norm_mix_g"][l:l + 1, :].partition_broadcast(128)[:, 0, :]),
                   writes=["gb"])
            pg.op("dve", lambda e: e.scalar_tensor_tensor(mod[:, 1024:1024 + D], mod[:, 1024:1024 + D], 1.0, gb, ALU.add, ALU.mult),
                  reads=["gb", "mod"], writes=["mod"])
            wv = [vaug[i // 2][:].rearrange("p a b -> p (a b)")[:, (i % 2) * 2048:(i % 2 + 1) * 2048].rearrange("p (k n) -> p k n", k=8)
                  for i in range(4)]
            bb = [rrow[:, 0:256], rrow[:, 256:512]]
            gb2 = xnt[0][:]

            def m0_load(e_):
                c0 = 2048 + e_ * 256
                pg.dma("pool", lambda e: e.dma_start(
                    out=wv[e_ % 4], in_=din["w_ada"][l].rearrange("(k p) n -> p k n", p=128)[:, :, c0:c0 + 256]),
                    writes=[("wv", e_ % 4)])
                pg.dma("sp", lambda e: e.dma_start(out=bb[e_ % 2], in_=din["b_ada"][l:l + 1, c0:c0 + 256].partition_broadcast(128)[:, 0, :]),
                       writes=[("bb", e_ % 2)])

            def m0_comp(e_):
                c0 = 2048 + e_ * 256
                pb, pk = ps_next()
                for k in range(8):
                    pg.op("pe", lambda e: e.matmul(pb[:, 0:256], cb[:, k, :], wv[e_ % 4][:, k, :], start=(k == 0), stop=(k == 7)),
                          reads=["cb", ("wv", e_ % 4)], writes=[pk])
                pg.op("dve", lambda e: e.tensor_tensor(mod[:, c0:c0 + 256], pb[:, 0:256], bb[e_ % 2], ALU.add),
                      reads=[pk, ("bb", e_ % 2)], writes=["mod"])
                if e_ == 11:
                    pg.dma("sp", lambda e: e.dma_start(out=gb2, in_=din["norm_ffn_g"][l:l + 1, :].partition_broadcast(128)[:, 0, :]),
                           writes=[("xnt", 0)])
                    pg.op("dve", lambda e: e.scalar_tensor_tensor(mod[:, 4096:4096 + D], mod[:, 4096:4096 + D], 1.0, gb2, ALU.add, ALU.mult),
                          reads=[("xnt", 0), "mod"], writes=["mod"])
            m0_load(0)
            m0_load(1)
        SH1, G1, GTM = mod[:, 0:D], mod[:, D:2 * D], mod[:, 2 * D:3 * D]
        SH2, G2, GTF = mod[:, 3 * D:4 * D], mod[:, 4 * D:5 * D], mod[:, 5 * D:6 * D]
        if "dbg_mod" in dbg_d and l == 0:
            pg.dma("sp", lambda e: e.dma_start(out=dbg_d["dbg_mod"], in_=mod[:]), reads=["mod"], writes=["dbgmod"])

        w_in_b = arenaB[:].rearrange("p (k n) -> p k n", k=8)
        wsrc = din["w_in"][l].rearrange("(k p) n -> p k n", p=128)
        pg.dma("pool", lambda e: e.dma_start(out=w_in_b[:, :, 0:1024], in_=wsrc[:, :, 0:1024]), writes=["w_in_b"])
        pg.dma("pool", lambda e: e.dma_start(out=w_in_b[:, :, 1024:2056], in_=wsrc[:, :, 1024:2056]), writes=["w_in_b"])
        pg.dma("pool", lambda e: e.dma_start(out=wpool_b[:], in_=din["w_pool"][l].rearrange("g c d -> c g d")), writes=["wpool_b"])
        pg.dma("sp", lambda e: e.dma_start(out=pscol[:], in_=din["ps_col"][l]), writes=["pscol"])
        pg.dma("sp", lambda e: e.dma_start(out=bfg[:], in_=din["b_fgate"][l:l + 1, :].partition_broadcast(128)[:, 0, :]), writes=["bfg"])
        hT = [arenaA[:, i * 4096:(i + 1) * 4096].rearrange("p (k t) -> p k t", k=8) for i in range(2)]
        uring = arenaA[:, 8192:12288].rearrange("p (r n) -> p r n", r=8)
        vtile = [arenaA[:, 12288 + i * 512:12288 + (i + 1) * 512] for i in range(2)]
        qks = [arenaA[:, 13312 + i * 512:13312 + (i + 1) * 512] for i in range(2)]
        dft = [arenaA[:, 14336 + i * 512:14336 + (i + 1) * 512] for i in range(2)]
        def p1_L(i):
            xs_ = i % 3
            pg.dma("sp", lambda e: e.dma_start(out=xt[xs_][:], in_=xsrc[i * 128:(i + 1) * 128, :]),
                   reads=[("xres", i)], writes=[("xt", xs_)])

        def p1_A0(i):
            rms_sq(xt[i % 3][:], ("xt", i % 3), i)

        def p1_A1(i):
            rms_apply(xt[i % 3][:], ("xt", i % 3), i, G1, SH1, hb[i % 2][:], ("hb", i % 2), i % 2)

        def p1_A2(i):
            B, r = divmod(i, 4)
            pb, pk = ps_next()
            pTv = pb[:].bitcast(BF16).rearrange("p (k t) -> p k t", k=8)
            for k in range(8):
                pg.op("pe", lambda e: e.transpose(pTv[:, k, :], hb[i % 2][:, k * 128:(k + 1) * 128], ident_b[:]),
                      reads=[("hb", i % 2), "ident_b"], writes=[pk])
            pg.op("act", lambda e: e.activation(hT[B % 2][:, :, r * 128:(r + 1) * 128], pTv, AF.Copy),
                  reads=[pk], writes=[("hT", B % 2, r)])

        def p1_B(i):
            B, r = divmod(i, 4)
            pv, pvk = ps_next()
            pu, puk = ps_next()
            pf, pfk = ps_next()
            for (pp, ppk, c0, c1) in ((pv, pvk, 1024, 1536), (pu, puk, 1544, 2056), (pf, pfk, 1536, 1544)):
                for k in range(8):
                    pg.op("pe", lambda e: e.matmul(
                        pp[:, 0:c1 - c0], hT[B % 2][:, k, r * 128:(r + 1) * 128], w_in_b[:, k, c0:c1], start=(k == 0), stop=(k == 7)),
                        reads=[("hT", B % 2, r), "w_in_b"], writes=[ppk])
            vs = i % 2
            pg.op("dve", lambda e: e.tensor_copy(vtile[vs], pv[:]), reads=[pvk], writes=[("vtile", vs)])
            pg.dma("sp", lambda e: e.dma_start(out=v_d[i * 128:(i + 1) * 128, :], in_=vtile[vs]),
                   reads=[("vtile", vs)], writes=[("v_d", i)])
            us = i % 8
            pg.op("act", lambda e: e.activation(uring[:, us, :], pu[:], AF.Copy), reads=[puk], writes=[("uring", us)])
            pg.op("dve", lambda e: e.tensor_copy(zf[:, :, i], pf[:, 0:8]), reads=[pfk], writes=[("zf", i)])

        def p1_QK(B):
            for c in range(8):
                pq, pqk = ps_next()
                for k in range(8):
                    pg.op("pe", lambda e: e.matmul(pq[:], w_in_b[:, k, c * 128:(c + 1) * 128], hT[B % 2][:, k, :],
                                                   start=(k == 0), stop=(k == 7)),
                          reads=[("hT", B % 2, r_) for r_ in range(4)] + ["w_in_b"], writes=[pqk])
                qs = c % 2
                if c % 2 == 0:
                    pg.op("dve", lambda e: e.tensor_copy(qks[qs], pq[:]), reads=[pqk], writes=[("qks", qs)])
                else:
                    pg.op("act", lambda e: e.activation(qks[qs], pq[:], AF.Copy), reads=[pqk], writes=[("qks", qs)])
                dst = qT_d if c < 4 else kT_d
                h0 = (c % 4) * 2
                for hh in range(2):
                    pg.dma("sp", lambda e: e.dma_start(
                        out=dst[h0 + hh, 0:64, B * 512:(B + 1) * 512], in_=qks[qs][hh * 64:(hh + 1) * 64, :]),
                        reads=[("qks", qs)], writes=[("qkd", c, hh, B)])

        def p1_POOL(B):
            for g in range(4):
                w = POOL_W[g]
                pd, pdk = ps_next()
                for r in range(4):
                    i = B * 4 + r
                    bsel = g * 3 + (2 if i == 0 else 0)
                    pg.op("pe", lambda e: e.matmul(
                        pd[:, r * 128:(r + 1) * 128], uring[:, i % 8, g * 128:(g + 1) * 128], band_b[:, bsel, :], start=True, stop=(i == 0)),
                        reads=[("uring", i % 8), "band_b"], writes=[pdk])
                    if i > 0:
                        pg.op("pe", lambda e: e.matmul(
                            pd[:, r * 128:(r + 1) * 128], uring[:, (i - 1) % 8, g * 128:(g + 1) * 128], band_b[:, g * 3 + 1, :], start=False, stop=True),
                            reads=[("uring", (i - 1) % 8), "band_b"], writes=[pdk])
                ds = g % 2
                if B == 0:
                    pg.op("dve", lambda e: e.tensor_tensor(dft[ds][:, 0:128], pd[:, 0:128], invc_f[:, g, :], ALU.mult),
                          reads=[pdk, "invc_f"], writes=[("dft", ds)])
                    pg.op("act", lambda e: e.activation(dft[ds][:, 128:512], pd[:, 128:512], AF.Copy, scale=1.0 / w),
                          reads=[pdk], writes=[("dft", ds)])
                else:
                    pg.op("act", lambda e: e.activation(dft[ds], pd[:], AF.Copy, scale=1.0 / w),
                          reads=[pdk], writes=[("dft", ds)])
                pm, pmk = ps_next()
                pg.op("pe", lambda e: e.matmul(pm[:], wpool_b[:, g, :], dft[ds], start=True, stop=True),
                      reads=["wpool_b", ("dft", ds)], writes=[pmk])
                pg.op("act", lambda e: e.activation(catT[:, 4 + g, B * 512:(B + 1) * 512], pm[:], AF.Identity, scale=pscol[:, g:g + 1]),
                      reads=[pmk, "pscol"], writes=[("catT", 4 + g, B * 4 + r_) for r_ in range(4)])

        def p1_FG():
            zkeys = [("zf", i) for i in range(NT)]
            pg.op("dve", lambda e: e.tensor_tensor(zf[:], zf[:], bfg[:].unsqueeze(2).to_broadcast([128, 8, NT]), ALU.add),
                  reads=zkeys + ["bfg"], writes=["zfa"])
            pg.op("act", lambda e: e.activation(fA[:], zf[:], AF.Exp, scale=-1.0), reads=["zfa"], writes=["fA"])
            pg.op("act", lambda e: e.activation(fB[:], fA[:], AF.Ln, bias=1.0), reads=["fA"], writes=["fB"])
            pl, plk = ps_next()
            pt2, ptk = ps_next()
            nlf2 = fB[:].rearrange("p h t -> p (h t)")
            pg.op("pe", lambda e: e.matmul(pl[:, 0:256], triu_f[:], nlf2, start=True, stop=True), reads=["fB", "triu_f"], writes=[plk])
            pg.op("pe", lambda e: e.matmul(pt2[:, 0:256], ones_f[:], nlf2, start=True, stop=True), reads=["fB", "ones_f"], writes=[ptk])
            pg.op("dve", lambda e: e.tensor_copy(ftot[:].rearrange("p h t -> p (h t)"), pt2[:, 0:256]), reads=[ptk], writes=["ftot"])
            pg.op("dve", lambda e: e.tensor_copy(fA[:], ftot[:]), reads=["ftot", "fA"], writes=["fA"])
            cur, oth, ck, ok_ = fA, fB, "fA", "fB"
            for sft in (1, 2, 4, 8, 16):
                pg.op("dve", lambda e, cur=cur, oth=oth, sft=sft: e.tensor_copy(oth[:, :, 0:sft], cur[:, :, 0:sft]),
                      reads=[ck, plk, ptk], writes=[ok_])
                pg.op("dve", lambda e, cur=cur, oth=oth, sft=sft: e.tensor_tensor(oth[:, :, sft:NT], cur[:, :, sft:NT], cur[:, :, 0:NT - sft], ALU.add),
                      reads=[ck], writes=[ok_])
                cur, oth, ck, ok_ = oth, cur, ok_, ck
            pg.op("dve", lambda e, cur=cur: e.tensor_tensor(Gm[:], cur[:], ftot[:], ALU.subtract), reads=[ck, "ftot"], writes=["Gm"])
            pg.op("dve", lambda e: e.tensor_tensor(Gm[:].rearrange("p h t -> p (h t)"), Gm[:].rearrange("p h t -> p (h t)"), pl[:, 0:256], ALU.add),
                  reads=["Gm", plk], writes=["Gm"])
            if "dbg_G" in dbg_d and l == 0:
                pg.dma("sp", lambda e: e.dma_start(out=dbg_d["dbg_G"], in_=Gm[:]), reads=["Gm"], writes=["dbgG"])
            for half in range(2):
                pt3, pt3k = ps_next()
                pg.op("pe", lambda e, pt3=pt3, half=half: e.transpose(pt3[:, 0:128], Gm[:, half * 4:(half + 1) * 4, :].rearrange("p h t -> p (h t)"), ident_f[:]),
                      reads=["Gm", "ident_f"], writes=[pt3k])
                pg.op("act", lambda e, pt3=pt3, half=half: e.activation(gtn[half][:], pt3[:, 0:128], AF.Copy, scale=-1.0),
                      reads=[pt3k], writes=[("gtn", half)])
                for hl in range(4):
                    h = half * 4 + hl
                    pg.dma("sp", lambda e, h=h, hl=hl, half=half: e.dma_start(
                        out=qT_d[h, 64, :].rearrange("(i t) -> i t", t=128), in_=gtn[half][hl * 32:(hl + 1) * 32, :]),
                        reads=[("gtn", half)], writes=[("qaug", h)])

        for it in range(-3, NT + 1):
            if 0 <= it + 3 < NT:
                p1_L(it + 3)
            if 0 <= it + 2 < NT:
                p1_A0(it + 2)
            if 0 <= it + 1 < NT:
                p1_A1(it + 1)
            if 0 <= it < NT:
                p1_A2(it)
            if l == 0 and 0 <= it < NT and it % 2 == 0:
                m0_comp(it // 2)
                if it // 2 + 2 < 16:
                    m0_load(it // 2 + 2)
            if 0 <= it - 1 < NT:
                p1_B(it - 1)
                if it - 1 == NT - 1:
                    p1_FG()
                if (it - 1) % 4 == 3:
                    p1_QK((it - 1) // 4)
                    p1_POOL((it - 1) // 4)
        if stop == "p1":
            if "dbg_cat" in dbg_d:
                pg.dma("sp", lambda e: e.dma_start(out=dbg_d["dbg_cat"], in_=catT), reads=[("catT", c, i) for c in range(8) for i in range(NT)], writes=["dbgcat"])
            break

        pg.fence(fsc[:, 1:2])
        if l == 0:
            for i in range(2):
                col = 0 if i % 2 else 64
                pg.op("pool", lambda e: e.memset(vaug[i][:, :, col:col + 64], 1.0),
                      writes=[("vaug", i), ("wv", 2 * i), ("wv", 2 * i + 1)])
        qkh = arenaB[:].rearrange("p (s n) -> p s n", s=4)
        PT = [hb[0][:, 0:512], hb[0][:, 512:1024], hb[1][:, 0:512], hb[1][:, 512:1024]]
        po = [psb[0], psb[1], psb[2]]
        NSR = 4
        s_ring = [psb[3 + i] for i in range(NSR)]
        do_m = (l + 1 < nl)
        LA = 2
        all_qk_keys = [("qkd", c, hh, B) for c in range(8) for hh in range(2) for B in range(8)] + [("qaug", h) for h in range(8)] + ["kaug"]

        def load_head(h):
            sl = h % 2
            vs = h % 2
            pg.dma("sp", lambda e: e.dma_start(out=qkh[0:65, sl * 2 + 0, 0:S], in_=qT_d[h]), reads=all_qk_keys, writes=[("qh", sl)])
            pg.dma("sp", lambda e: e.dma_start(out=qkh[0:65, sl * 2 + 1, 0:S], in_=kT_d[h]), reads=all_qk_keys, writes=[("kh", sl)])
            off = 64 if h % 2 else 0
            pg.dma("sp", lambda e: e.dma_start(out=vaug[vs][:, :, off:off + 64], in_=v_d.rearrange("(i p) c -> p i c", p=128)[:, :, h * 64:(h + 1) * 64]),
                   reads=[("v_d", i) for i in range(NT)], writes=[("vaug", vs)])

        tiles = []
        blk = 0
        for h in range(NH):
            for I in range(8):
                n_kt = 4 * (I + 1)
                for j in range(n_kt - 1, -1, -1):
                    tiles.append((h, I, j, blk, j == n_kt - 1, j == 0))
                blk += 1

        def emit_S(n):
            h, I, j, blk_, first, last = tiles[n]
            sl = h % 2
            qh = qkh[0:65, sl * 2 + 0, 0:S]
            kh = qkh[0:65, sl * 2 + 1, 0:S]
            r = j - 4 * I
            q0 = max(r, 0) * 128
            sb_ = s_ring[n % NSR]
            sk = ("ps", 3 + n % NSR)
            kt = kh[:, j * 128:(j + 1) * 128]
            rd = [("qh", sl), ("kh", sl)]
            if r >= 0:
                pg.op("pe", lambda e: e.matmul(sb_[:, q0:q0 + 128], kt, qh[:, I * 512 + q0:I * 512 + q0 + 128], start=True, stop=False),
                      reads=rd, writes=[sk])
                pg.op("pe", lambda e: e.matmul(sb_[:, q0:q0 + 128], negm_b[:], ident_b[:], start=False, stop=True),
                      reads=["negm_b", "ident_b"], writes=[sk])
                if q0 + 128 < 512:
                    pg.op("pe", lambda e: e.matmul(sb_[:, q0 + 128:512], kt, qh[:, I * 512 + q0 + 128:(I + 1) * 512], start=True, stop=True),
                          reads=rd, writes=[sk])
            else:
                pg.op("pe", lambda e: e.matmul(sb_[:], kt, qh[:, I * 512:(I + 1) * 512], start=True, stop=True), reads=rd, writes=[sk])

        def emit_exp_pv(n):
            h, I, j, blk_, first, last = tiles[n]
            sl = h % 2
            vs = h % 2
            qh = qkh[0:65, sl * 2 + 0, 0:S]
            r = j - 4 * I
            q0 = max(r, 0) * 128
            sb_ = s_ring[n % NSR]
            sk = ("ps", 3 + n % NSR)
            ptl = PT[n % 4]
            ptk_ = ("PT", n % 4)
            pob = po[blk_ % 3]
            pok = ("ps", blk_ % 3)
            if first:
                pg.op("pe", lambda e: e.matmul(pob[:], zero_b[0:65, :], qh[:, I * 512:(I + 1) * 512], start=True, stop=False),
                      reads=["zero_b", ("qh", sl)], writes=[pok])
            pg.op("act", lambda e: e.activation(ptl[:, q0:512], sb_[:, q0:512], AF.Exp, bias=Gm[:, h, j:j + 1], scale=0.125),
                  reads=[sk, "Gm"], writes=[ptk_])
            pg.op("pe", lambda e: e.matmul(pob[:, q0:512], vaug[vs][:, j, :], ptl[:, q0:512], start=False, stop=last),
                  reads=[("vaug", vs), ptk_], writes=[pok])

        def emit_norm(h, I, blk_):
            odd = h % 2
            rows = slice(64, 128) if odd else slice(0, 64)
            drows = slice(0, 64) if odd else slice(64, 128)
            pob = po[blk_ % 3]
            rr = tmpf[(blk_ % 3) // 2][:, ((blk_ % 3) % 2) * 512:((blk_ % 3) % 2 + 1) * 512]
            pg.op("dve", lambda e: e.reciprocal(rr[drows, :], pob[drows, :]), reads=[("ps", blk_ % 3)], writes=[("rrow", blk_ % 3)])
            pg.op("dve", lambda e: e.tensor_tensor(catT[rows, h // 2, I * 512:(I + 1) * 512], pob[rows, :], rr[drows, :], ALU.mult),
                  reads=[("ps", blk_ % 3), ("rrow", blk_ % 3)], writes=[("catT", h // 2, I * 4 + r_, odd) for r_ in range(4)])

        load_head(0)
        load_head(1)
        ntl = len(tiles)
        for n in range(min(LA, ntl)):
            emit_S(n)
        for n in range(ntl):
            h, I, j, blk_, first, last = tiles[n]
            if n + LA < ntl:
                emit_S(n + LA)
            if first and (blk_ % 2 == 0):
                conv_step(3)
            if first and (blk_ % 2 == 1) and do_m:
                ev = blk_ // 2
                if ev == 0:
                    m_load(l + 1, 0)
                    m_load(l + 1, 1)
                if ev < NMC:
                    m_comp(ev, 7)
                    if ev + 2 < NMC:
                        m_load(l + 1, ev + 2)
            emit_exp_pv(n)
            if last:
                emit_norm(h, I, blk_)
                if I == 7 and h + 2 < NH:
                    load_head(h + 2)
        if stop == "p2":
            if "dbg_cat" in dbg_d:
                pg.dma("sp", lambda e: e.dma_start(out=dbg_d["dbg_cat"], in_=catT),
                       reads=[("catT", c, i) for c in range(4, 8) for i in range(NT)] + [("catT", c, i, o_) for c in range(4) for i in range(NT) for o_ in range(2)],
                       writes=["dbgcat"])
            break

        pg.fence(fsc[:, 2:3])
        w_out_b = arenaB[:, 0:8 * D].rearrange("p (k n) -> p k n", k=8)
        pg.dma("pool", lambda e: e.dma_start(out=w_out_b, in_=din["w_out"][l].rearrange("(k p) n -> p k n", p=128)), writes=["w_out_b"])
        pg.dma("pool", lambda e: e.dma_start(out=wr_b[:], in_=din["w_r"][l].rearrange("(k p) n -> p k n", p=128)), writes=["wr_b"])
        pg.dma("sp", lambda e: e.dma_start(out=brb[:], in_=din["b_r"][l:l + 1, :].partition_broadcast(128)[:, 0, :]), writes=["brb"])
        hT2 = [arenaB[:, 8192 + i * 1024:8192 + (i + 1) * 1024].rearrange("p (k t) -> p k t", k=8) for i in range(2)]
        p3_pm = {}

        def p3_M(i):
            xs_ = i % 3
            pg.dma("sp", lambda e: e.dma_start(out=xt[xs_][:], in_=xsrc[i * 128:(i + 1) * 128, :]), reads=[("xres", i)], writes=[("xt", xs_)])
            cat_keys = [("catT", c, i) for c in range(4, 8)] + [("catT", c, i, o_) for c in range(4) for o_ in range(2)]
            pmx = []
            for half in range(2):
                bnk = (i % 3) * 2 + half
                pm_, pmk_ = psb[bnk], ("ps", bnk)
                pmx.append((pm_, pmk_))
                for c in range(8):
                    pg.op("pe", lambda e: e.matmul(
                        pm_[:], catT[:, c, i * 128:(i + 1) * 128], w_out_b[:, c, half * 512:(half + 1) * 512], start=(c == 0), stop=(c == 7)),
                        reads=cat_keys + ["w_out_b"], writes=[pmk_])
            p3_pm[i] = pmx

        def p3_R1(i):
            xs_ = i % 3
            xn_ = i % 2
            pmx = p3_pm.pop(i)
            for half in range(2):
                pm_, pmk_ = pmx[half]
                pg.op("dve", lambda e: e.tensor_tensor(
                    xnt[xn_][:, half * 512:(half + 1) * 512], pm_[:], GTM[:, half * 512:(half + 1) * 512], ALU.mult),
                    reads=[pmk_, "mod"], writes=[("xnt", xn_)])
            pg.op("dve", lambda e: e.tensor_tensor(xnt[xn_][:], xnt[xn_][:], xt[xs_][:], ALU.add),
                  reads=[("xnt", xn_), ("xt", xs_)], writes=[("xnt", xn_)])
            pg.dma("sp", lambda e: e.dma_start(out=xres[i * 128:(i + 1) * 128, :], in_=xnt[xn_][:]),
                   reads=[("xnt", xn_)], writes=[("xres", i)])
            rms_sq(xnt[xn_][:], ("xnt", xn_), i)

        def p3_R2(i):
            xn_ = i % 2
            rms_apply(xnt[xn_][:], ("xnt", xn_), i, G2, SH2, hb[i % 2][:], ("hb", i % 2), i % 2)
            pg.dma("sp", lambda e: e.dma_start(out=h_d[i * 128:(i + 1) * 128, :], in_=hb[i % 2][:]),
                   reads=[("hb", i % 2)], writes=[("h_d", i)])

        def p3_R3(i):
            pb, pk = psb[6], ("ps", 6)
            pTv = pb[:].bitcast(BF16).rearrange("p (k t) -> p k t", k=8)
            for k in range(8):
                pg.op("pe", lambda e: e.transpose(pTv[:, k, :], hb[i % 2][:, k * 128:(k + 1) * 128], ident_b[:]),
                      reads=[("hb", i % 2), "ident_b"], writes=[pk])
            pg.op("act", lambda e: e.activation(hT2[i % 2], pTv, AF.Copy), reads=[pk], writes=[("hT2", i % 2)])

        def p3_LG(i):
            plg, plgk = psb[7], ("ps", 7)
            for k in range(8):
                pg.op("pe", lambda e: e.matmul(plg[:, 0:36], hT2[i % 2][:, k, :], wr_b[:, k, :], start=(k == 0), stop=(k == 7)),
                      reads=[("hT2", i % 2), "wr_b"], writes=[plgk])
            pg.op("dve", lambda e: e.tensor_tensor(LG[:, i, :], plg[:, 0:36], brb[:], ALU.add),
                  reads=[plgk, "brb"], writes=[("LG", i)])

        for i in range(min(3, NT)):
            p3_M(i)
        for it in range(NT + 3):
            if it < NT:
                p3_R1(it)
            if it + 3 < NT:
                p3_M(it + 3)
            if 0 <= it - 1 < NT:
                p3_R2(it - 1)
            if 0 <= it - 2 < NT:
                p3_R3(it - 2)
            if 0 <= it - 3 < NT:
                p3_LG(it - 3)
        if "dbg_LG" in dbg_d and l == 0:
            pg.dma("sp", lambda e: e.dma_start(out=dbg_d["dbg_LG"], in_=LG[:]), reads=[("LG", i) for i in range(NT)], writes=["dbgLG"])

        pg.fence(fsc[:, 3:4])
        RA = arenaA[:].bitcast(F32)

        def rt(idx_, n=1024):
            return RA[:, idx_ * 1024:idx_ * 1024 + n]
        vm = rt(0).rearrange("p (i g) -> p i g", i=NT)
        vm4 = rt(0).rearrange("p (i g e) -> p i g e", i=NT, g=4)
        oh1 = rt(1).rearrange("p (i g) -> p i g", i=NT)
        oh2 = rt(2).rearrange("p (i g) -> p i g", i=NT)
        vm2 = rt(3).rearrange("p (i g) -> p i g", i=NT)
        tA = rt(4).rearrange("p (i g) -> p i g", i=NT)
        tB = rt(5).rearrange("p (i g) -> p i g", i=NT)
        rin = rt(6).rearrange("p (i g) -> p i g", i=NT)
        tot = rt(7).rearrange("p (i g) -> p i g", i=NT)
        cmp3 = RA[:, 8 * 1024:8 * 1024 + NTILE * 32].rearrange("p (j e) -> p j e", e=32)
        sm = RA[:, 11 * 1024:12 * 1024]
        ohb = arenaA[:, 12 * 2048:12 * 2048 + 1024]
        gmax, sumg, topp = sm[:, 0:32], sm[:, 32:64], sm[:, 64:96]
        m1, m2, e2 = sm[:, 96:128], sm[:, 128:160], sm[:, 160:192]
        ew1, pos1f, pos2f = sm[:, 192:224], sm[:, 224:256], sm[:, 256:288]
        cntv, pcv, t1v = sm[:, 288:320], sm[:, 320:352], sm[:, 352:384]
        offA, offB, endo = sm[:, 384:416], sm[:, 416:448], sm[:, 448:480]
        ohg = sm[:, 480:608].rearrange("p (i g) -> p i g", g=4)
        egx = sm[:, 608:736].rearrange("p (i g) -> p i g", g=4)
        pen = sm[:, 736:864].rearrange("p (i g) -> p i g", g=4)
        tef = sm[:, 864:960]
        LGg = LG[:, :, 0:4]
        LE4 = LG[:, :, 4:36].rearrange("p i (g e) -> p i g e", g=4)
        RK = "route"
        lgk = [("LG", i) for i in range(NT)]
        NSL = 8
        hsl = [arenaB[:, i * 1024:(i + 1) * 1024] for i in range(NSL)]

        def hsl_load(i):
            pg.dma("sp", lambda e: e.dma_start(out=hsl[i % NSL], in_=h_d[i * 128:(i + 1) * 128, :]),
                   reads=[("h_d", i)], writes=[("hsl", i % NSL)])
        for i in range(NSL):
            hsl_load(i)

        def dv(fn, reads=(), eng="dve"):
            pg.op(eng, fn, reads=[RK] + list(reads), writes=[RK])
        dv(lambda e: e.tensor_reduce(gmax, LGg, AX.X, ALU.max), reads=lgk)
        dv(lambda e: e.tensor_tensor(ohg, LGg, gmax.unsqueeze(2).to_broadcast([128, NT, 4]), ALU.is_equal))
        dv(lambda e: e.tensor_tensor(egx, LGg, gmax.unsqueeze(2).to_broadcast([128, NT, 4]), ALU.subtract))
        dv(lambda e: e.activation(egx, egx, AF.Exp), eng="act")
        dv(lambda e: e.tensor_reduce(sumg, egx, AX.X, ALU.add))
        dv(lambda e: e.reciprocal(topp, sumg))
        dv(lambda e: e.tensor_scalar(pen, ohg, BIG, -BIG, ALU.mult, ALU.add))
        dv(lambda e: e.tensor_tensor(vm4, LE4, pen.unsqueeze(3).to_broadcast([128, NT, 4, 8]), ALU.add))
        dv(lambda e: e.tensor_reduce(m1, vm, AX.X, ALU.max))
        dv(lambda e: e.tensor_tensor(oh1, vm, m1.unsqueeze(2).to_broadcast([128, NT, 32]), ALU.is_equal))
        dv(lambda e: e.scalar_tensor_tensor(vm2, oh1, -BIG, vm, ALU.mult, ALU.add))
        dv(lambda e: e.tensor_reduce(m2, vm2, AX.X, ALU.max))
        dv(lambda e: e.tensor_tensor(oh2, vm2, m2.unsqueeze(2).to_broadcast([128, NT, 32]), ALU.is_equal))
        dv(lambda e: e.tensor_tensor(e2, m2, m1, ALU.subtract))
        dv(lambda e: e.activation(e2, e2, AF.Exp), eng="act")
        dv(lambda e: e.tensor_scalar(ew1, e2, 1.0, None, ALU.add))
        dv(lambda e: e.reciprocal(ew1, ew1))
        dv(lambda e: e.tensor_tensor(comb[0][:], topp, ew1, ALU.mult))
        dv(lambda e: e.tensor_tensor(comb[1][:], comb[0][:], e2, ALU.mult))
        dv(lambda e: e.tensor_tensor(ohb.rearrange("p (i g) -> p i g", i=NT), oh1, oh2, ALU.add))
        for (dst, lhs, lk) in ((rin, ltri_b, "ltri_b"), (tot, ones_b, "ones_b")):
            for half in range(2):
                pb, pk = ps_next()
                pg.op("pe", lambda e, pb=pb, lhs=lhs, half=half: e.matmul(pb[:], lhs[:], ohb[:, half * 512:(half + 1) * 512], start=True, stop=True),
                      reads=[RK, lk], writes=[pk])
                pg.op("dve", lambda e, pb=pb, dst=dst, half=half: e.tensor_copy(dst[:, half * 16:(half + 1) * 16, :].rearrange("p i g -> p (i g)"), pb[:]),
                      reads=[pk, RK], writes=[RK])
        dv(lambda e: e.tensor_copy(tA, tot))
        cur, oth = tA, tB
        for sft in (1, 2, 4, 8, 16):
            dv(lambda e, cur=cur, oth=oth, sft=sft: e.tensor_copy(oth[:, 0:sft, :], cur[:, 0:sft, :]))
            dv(lambda e, cur=cur, oth=oth, sft=sft: e.tensor_tensor(oth[:, sft:NT, :], cur[:, sft:NT, :], cur[:, 0:NT - sft, :], ALU.add))
            cur, oth = oth, cur
        inc3 = cur
        dv(lambda e: e.tensor_copy(cntv, inc3[:, NT - 1, :]))
        dv(lambda e: e.tensor_scalar(t1v, cntv, 127.0, None, ALU.add))
        dv(lambda e: e.tensor_scalar(pcv, t1v, 1.0 / 128, -0.49609375, ALU.mult, ALU.add))
        dv(lambda e: e.tensor_scalar(pcv, pcv, 8388608.0, None, ALU.add))
        dv(lambda e: e.tensor_scalar(pcv, pcv, -8388608.0, 128.0, ALU.add, ALU.mult))
        dv(lambda e: e.tensor_copy(offA, pcv))
        c2, o2 = offA, offB
        for sft in (1, 2, 4, 8, 16):
            dv(lambda e, c2=c2, o2=o2, sft=sft: e.tensor_copy(o2[:, 0:sft], c2[:, 0:sft]))
            dv(lambda e, c2=c2, o2=o2, sft=sft: e.tensor_tensor(o2[:, sft:32], c2[:, sft:32], c2[:, 0:32 - sft], ALU.add))
            c2, o2 = o2, c2
        dv(lambda e, c2=c2: e.tensor_copy(endo, c2))
        dv(lambda e: e.tensor_tensor(offA if c2 is not offA else offB, endo, pcv, ALU.subtract))
        offx = offA if c2 is not offA else offB
        dv(lambda e: e.tensor_tensor(oth, inc3, tot, ALU.subtract))
        dv(lambda e: e.tensor_tensor(oth, oth, rin, ALU.add))
        dv(lambda e: e.tensor_tensor(oth, oth, offx.unsqueeze(1).to_broadcast([128, NT, 32]), ALU.add))
        dv(lambda e: e.tensor_tensor(vm, oth, oh1, ALU.mult))
        dv(lambda e: e.tensor_reduce(pos1f, vm, AX.X, ALU.add))
        dv(lambda e: e.tensor_tensor(vm, oth, oh2, ALU.mult))
        dv(lambda e: e.tensor_reduce(pos2f, vm, AX.X, ALU.add))
        dv(lambda e: e.tensor_copy(pos_i[0][:], pos1f))
        dv(lambda e: e.tensor_copy(pos_i[1][:], pos2f))
        dv(lambda e: e.tensor_tensor(cmp3, endo.unsqueeze(1).to_broadcast([128, NTILE, 32]), jv_f[:].unsqueeze(2).to_broadcast([128, NTILE, 32]), ALU.is_le),
           reads=["jv_f"])
        dv(lambda e: e.tensor_reduce(tef, cmp3, AX.X, ALU.add))
        dv(lambda e: e.tensor_scalar(tef, tef, 128.0, None, ALU.mult))
        dv(lambda e: e.tensor_scalar(tef, tef, pidx_f[:, 0:1], None, ALU.add), reads=["pidx_f"])
        dv(lambda e: e.tensor_copy(idxW[:], tef))
        if "dbg_route" in dbg_d and l == 0:
            dr = dbg_d["dbg_route"]
            for n_, src_ in enumerate((pos1f, pos2f, comb[0][:], comb[1][:], m1, m2)):
                pg.dma("sp", lambda e, n_=n_, src_=src_: e.dma_start(out=dr[:, n_, :], in_=src_), reads=[RK], writes=[("dbgr", n_)])
            if "dbg_te" in dbg_d:
                pg.dma("sp", lambda e: e.dma_start(out=dbg_d["dbg_te"], in_=tef), reads=[RK], writes=["dbgte"])
        for i in range(NT):
            for k in range(2):
                pg.dma("pool", lambda e: e.indirect_dma_start(
                    out=hs_d, out_offset=bass.IndirectOffsetOnAxis(ap=pos_i[k][:, i:i + 1], axis=0), in_=hsl[i % NSL], in_offset=None),
                    reads=[("hsl", i % NSL), RK], writes=[("hs_d", i, k)])
            if i + NSL < NT:
                hsl_load(i + NSL)
        if stop == "p3":
            break

        pg.fence(fsc[:, 4:5])
        NW = 5
        Wt = [arenaA[:, i * 6144:(i + 1) * 6144] for i in range(NW)]
        o4 = 0
        hst = [arenaB[:, o4 + i * 1024:o4 + (i + 1) * 1024] for i in range(3)]
        o4 += 3 * 1024
        hsT = [arenaB[:, o4 + i * 1024:o4 + (i + 1) * 1024].rearrange("p (k t) -> p k t", k=8) for i in range(3)]
        o4 += 3 * 1024
        sa = [arenaB[:, o4 + i * 512:o4 + (i + 1) * 512].bitcast(F32) for i in range(3)]
        o4 += 3 * 512
        actb = [arenaB[:, o4 + i * 256:o4 + (i + 1) * 256] for i in range(3)]
        o4 += 3 * 256
        actT = [arenaB[:, o4 + i * 256:o4 + (i + 1) * 256].rearrange("p (k t) -> p k t", k=2) for i in range(3)]
        o4 += 3 * 256
        yt = [xnt[0], xnt[1]]
        hs_keys = [("hs_d", i, k) for i in range(NT) for k in range(2)]
        wbf_keys = [("wbf", l, e_, p_) for e_ in range(32) for p_ in range(3)]
        while conv_items and conv_items[0][0] <= l:
            conv_step(1)

        def moe_load(j):
            pg.dma("pool", lambda e: e.indirect_dma_start(
                out=Wt[j % NW], out_offset=None, in_=wbf_d[l], in_offset=bass.IndirectOffsetOnAxis(ap=idxW[:, j:j + 1], axis=0),
                bounds_check=pg.wbound, oob_is_err=False),
                reads=wbf_keys + [RK], writes=[("Wt", j % NW)])
            pg.dma("sp", lambda e: e.dma_start(out=hst[j % 3], in_=hs_d[j * 128:(j + 1) * 128, :]),
                   reads=hs_keys, writes=[("hst", j % 3)])

        def moe_S1(j):
            pb, pk = ps_next()
            pTv = pb[:].bitcast(BF16).rearrange("p (k t) -> p k t", k=8)
            for k in range(8):
                pg.op("pe", lambda e: e.transpose(pTv[:, k, :], hst[j % 3][:, k * 128:(k + 1) * 128], ident_b[:]),
                      reads=[("hst", j % 3), "ident_b"], writes=[pk])
            pg.op("act", lambda e: e.activation(hsT[j % 3], pTv, AF.Copy), reads=[pk], writes=[("hsT", j % 3)])

        def moe_S2(j):
            w_ = Wt[j % NW]
            pgu, pguk = ps_next()
            for k in range(8):
                pg.op("pe", lambda e: e.matmul(pgu[:], hsT[j % 3][:, k, :], w_[:, k * 512:(k + 1) * 512], start=(k == 0), stop=(k == 7)),
                      reads=[("hsT", j % 3), ("Wt", j % NW)], writes=[pguk])
            pg.op("act", lambda e: e.activation(sa[j % 3], pgu[:, 0:256], AF.Silu), reads=[pguk], writes=[("sa", j % 3)])
            pg.op("dve", lambda e: e.tensor_tensor(actb[j % 3], sa[j % 3], pgu[:, 256:512], ALU.mult),
                  reads=[pguk, ("sa", j % 3)], writes=[("actb", j % 3)])

        def moe_S3(j):
            pb2, pk2 = ps_next()
            pT2 = pb2[:].bitcast(BF16)[:, 0:256].rearrange("p (k t) -> p k t", k=2)
            for k in range(2):
                pg.op("pe", lambda e: e.transpose(pT2[:, k, :], actb[j % 3][:, k * 128:(k + 1) * 128], ident_b[:]),
                      reads=[("actb", j % 3), "ident_b"], writes=[pk2])
            pg.op("dve", lambda e: e.tensor_copy(actT[j % 3], pT2), reads=[pk2], writes=[("actT", j % 3)])

        def moe_S4(j):
            w_ = Wt[j % NW]
            for half in range(2):
                py, pyk = ps_next()
                for k in range(2):
                    pg.op("pe", lambda e: e.matmul(
                        py[:], actT[j % 3][:, k, :], w_[:, 4096 + k * 1024 + half * 512:4096 + k * 1024 + (half + 1) * 512], start=(k == 0), stop=(k == 1)),
                        reads=[("actT", j % 3), ("Wt", j % NW)], writes=[pyk])
                pg.op("dve", lambda e: e.tensor_tensor(yt[j % 2][:, half * 512:(half + 1) * 512], py[:], GTF[:, half * 512:(half + 1) * 512], ALU.mult),
                      reads=[pyk, "mod"], writes=[("yt", j % 2, half)])
            pg.dma("sp", lambda e: e.dma_start(out=ys_d[j * 128:(j + 1) * 128, :], in_=yt[j % 2][:]),
                   reads=[("yt", j % 2, 0), ("yt", j % 2, 1)], writes=[("ys_d", j)])

        PF = 2
        for j in range(min(PF, NTILE)):
            moe_load(j)
        for s_ in range(NTILE + 3):
            if 0 <= s_ - 3 < NTILE:
                moe_S4(s_ - 3)
            if s_ + PF < NTILE:
                moe_load(s_ + PF)
            if s_ < NTILE:
                moe_S1(s_)
            if 0 <= s_ - 1 < NTILE:
                moe_S2(s_ - 1)
            if 0 <= s_ - 2 < NTILE:
                moe_S3(s_ - 2)
        if stop == "p4":
            break

        pg.fence(fsc[:, 5:6])
        ys_keys = [("ys_d", j) for j in range(NTILE)]
        y12 = [[arenaA[:, (2 * s_ + k) * 2048:(2 * s_ + k + 1) * 2048].bitcast(F32) for k in range(2)] for s_ in range(3)]
        last = (l == nl - 1)
        if last:
            pg.dma("sp", lambda e: e.dma_start(out=gb, in_=din["norm_final_g"][0:1, :].partition_broadcast(128)[:, 0, :]), writes=["gb"])
        else:
            mod_reload(l + 1)

        def comb_load(i):
            for k in range(2):
                pg.dma("pool", lambda e, i=i, k=k: e.indirect_dma_start(
                    out=y12[i % 3][k], out_offset=None, in_=ys_d, in_offset=bass.IndirectOffsetOnAxis(ap=pos_i[k][:, i:i + 1], axis=0)),
                    reads=ys_keys + [RK], writes=[("y12", i % 3, k)])
            pg.dma("sp", lambda e, i=i: e.dma_start(out=xt[i % 3][:], in_=xres[i * 128:(i + 1) * 128, :]),
                   reads=[("xres", i)], writes=[("xt", i % 3)])

        comb_load(0)
        comb_load(1)
        for i in range(NT):
            if i + 2 < NT:
                comb_load(i + 2)
            ya, yb_ = y12[i % 3]
            ts = i % 2
            pg.op("act", lambda e, ya=ya, i=i, ts=ts: e.activation(tmpf[ts][:], ya, AF.Copy, scale=comb[0][:, i:i + 1]),
                  reads=[("y12", i % 3, 0), RK], writes=[("tmpf", ts)])
            pg.op("dve", lambda e, yb_=yb_, i=i, ts=ts: e.scalar_tensor_tensor(tmpf[ts][:], yb_, comb[1][:, i:i + 1], tmpf[ts][:], ALU.mult, ALU.add),
                  reads=[("y12", i % 3, 1), RK, ("tmpf", ts)], writes=[("tmpf", ts)])
            xn_ = i % 2
            pg.op("dve", lambda e, ts=ts, i=i, xn_=xn_: e.tensor_tensor(xnt[xn_][:], tmpf[ts][:], xt[i % 3][:], ALU.add),
                  reads=[("tmpf", ts), ("xt", i % 3)], writes=[("xnt", xn_)])
            if not last:
                pg.dma("sp", lambda e, xn_=xn_, i=i: e.dma_start(out=xres[i * 128:(i + 1) * 128, :], in_=xnt[xn_][:]),
                       reads=[("xnt", xn_)], writes=[("xres", i)])
            else:
                pg.op("act", lambda e, xn_=xn_, i=i: e.activation(sq[:], xnt[xn_][:], AF.Square, accum_out=ssq[:, i:i + 1]),
                      reads=[("xnt", xn_)], writes=["sq", ("ssq", i)])
                pg.op("dve", lambda e, i=i: e.tensor_scalar(rstd[:, i:i + 1], ssq[:, i:i + 1], 1.0 / D, EPS, ALU.mult, ALU.add),
                      reads=[("ssq", i)], writes=[("rstd", i)])
                pg.op("act", lambda e, i=i: e.activation(rstd[:, i:i + 1], rstd[:, i:i + 1], AF.Ln), reads=[("rstd", i)], writes=[("rstd", i)])
                pg.op("act", lambda e, i=i: e.activation(rstd[:, i:i + 1], rstd[:, i:i + 1], AF.Exp, scale=-0.5), reads=[("rstd", i)], writes=[("rstd", i)])
                pg.op("dve", lambda e, xn_=xn_, i=i: e.scalar_tensor_tensor(xnt[xn_][:], xnt[xn_][:], rstd[:, i:i + 1], gb, ALU.mult, ALU.mult),
                      reads=[("xnt", xn_), ("rstd", i), "gb"], writes=[("xnt", xn_)])
                pg.dma("sp", lambda e, xn_=xn_, i=i: e.dma_start(out=out_d[i * 128:(i + 1) * 128, :], in_=xnt[xn_][:]),
                       reads=[("xnt", xn_)], writes=[("out", i)])
    pg.emit()
    st.close()
    return nc, pg


def prep_inputs(inp):
    f = lambda a: np.ascontiguousarray(np.asarray(a, dtype=np.float32))
    shared = {}
    for k in ("norm_mix_g", "norm_ffn_g", "w_ada", "b_ada", "w_in", "b_fgate", "w_pool", "w_out"):
        shared[k] = f(inp[k])
    shared["norm_final_g"] = f(inp["norm_final_g"]).reshape(1, D)
    shared["ps_col"] = f(np.asarray(inp["pool_scale"]).reshape(NL, 4, 128).transpose(0, 2, 1))
    wre = np.asarray(inp["w_router_expert"]).transpose(0, 2, 1, 3).reshape(NL, D, 32)
    shared["w_r"] = f(np.concatenate([np.asarray(inp["w_router_group"]), wre], axis=2))
    shared["b_r"] = f(np.concatenate([np.asarray(inp["b_router_group"]), np.asarray(inp["b_router_expert"]).reshape(NL, 32)], axis=1))
    wg = np.asarray(inp["w_expert_gate"]).reshape(NL, 32, 8, 128, 256).transpose(0, 1, 3, 2, 4)
    wu = np.asarray(inp["w_expert_up"]).reshape(NL, 32, 8, 128, 256).transpose(0, 1, 3, 2, 4)
    wgu = np.concatenate([wg, wu], axis=4).reshape(NL, 32, 128, 4096)
    wd = np.asarray(inp["w_expert_down"]).reshape(NL, 32, 2, 128, 1024).transpose(0, 1, 3, 2, 4).reshape(NL, 32, 128, 2048)
    shared["wexp"] = f(np.concatenate([wgu, wd], axis=3).reshape(NL, 4096, 6144))
    for k, v in make_consts().items():
        shared["k_" + k] = f(v)
    x = np.asarray(inp["x"], dtype=np.float32)
    c = np.asarray(inp["c"], dtype=np.float32)
    per_core = []
    for b in range(8):
        m = dict(shared)
        m["x"] = np.ascontiguousarray(x[b])
        m["c_col"] = np.ascontiguousarray(c[b].reshape(8, 128).T)
        per_core.append(m)
    return per_core


_CACHE = {}


def kernel(**inputs):
    if "nc" not in _CACHE:
        _CACHE["nc"] = build()[0]
    nc = _CACHE["nc"]
    in_maps = prep_inputs(inputs)
    res = run_bass_kernel_spmd(nc, in_maps, core_ids=list(range(8)))
    return np.stack([np.asarray(r["out"], dtype=np.float32) for r in res.results], axis=0)
```
